# Optimizing a Trainium2 kernel written in Bass

```python
import jax
import jax.numpy as jnp
from jax import lax
import numpy as np

D_MODEL = 1024
BATCH = 8
SEQ = 2048
DEPTH = 2

CHUNK = 64
Q_BLOCK = 128
ROPE_BASE = 10000.0
LN_EPS = 1e-5
NEG = -1e30

RET_HEADS = 4
RET_DIM = 64
RET_W = RET_HEADS * RET_DIM
MLA_HEADS = 8
MLA_NOPE = 64
MLA_ROPE = 32
MLA_V = 64
MLA_Q_RANK = 256
MLA_KV_RANK = 128
MLA_W = MLA_HEADS * MLA_V
MLSTM_HEADS = 4
MLSTM_DIM = 64
MLSTM_W = MLSTM_HEADS * MLSTM_DIM
MLSTM_CONV = 4
MIX_W = RET_W + MLA_W + MLSTM_W

IN_SIZES = (RET_W, RET_W, RET_W, RET_W, MLA_Q_RANK, MLA_KV_RANK, MLA_ROPE, MLSTM_W, MLSTM_W, MLSTM_W, MLSTM_W, MLSTM_HEADS, MLSTM_HEADS)
IN_IS_VALUE = (False, False, True, False, False, False, False, False, False, True, False, False, False)
D_IN = sum(IN_SIZES)

D_FF = 2816
N_EXPERTS = 8
TOP_K = 2
D_FF_EXPERT = 3584
N_DENSE = (DEPTH + 1) // 2
N_MOE = DEPTH // 2

DN_ALPHA = (2 * DEPTH) ** 0.25
DN_BETA = (8 * DEPTH) ** -0.25

kernel_name = 'hybrid_retention_mla_mlstm_moe_block'


def layer_norm(x, w, b):
    x32 = x.astype(jnp.float32)
    mu = x32.mean(-1, keepdims=True)
    var = jnp.square(x32 - mu).mean(-1, keepdims=True)
    return ((x32 - mu) * lax.rsqrt(var + LN_EPS) * w + b).astype(x.dtype)


def rms_norm(x, w):
    x32 = x.astype(jnp.float32)
    y = x32 * lax.rsqrt(jnp.square(x32).mean(-1, keepdims=True) + LN_EPS) * w
    return y.astype(x.dtype)


def head_norm(x, w, n_heads):
    b, s, wd = x.shape
    xh = x.astype(jnp.float32).reshape(b, s, n_heads, wd // n_heads)
    mu = xh.mean(-1, keepdims=True)
    var = jnp.square(xh - mu).mean(-1, keepdims=True)
    return ((xh - mu) * lax.rsqrt(var + LN_EPS)).reshape(b, s, wd) * w


def to_heads(x, n):
    b, s, w = x.shape
    return x.reshape(b, s, n, w // n).transpose(0, 2, 1, 3)


def from_heads(x):
    b, h, s, d = x.shape
    return x.transpose(0, 2, 1, 3).reshape(b, s, h * d)


def rope(x, pos):
    half = x.shape[-1] // 2
    inv = ROPE_BASE ** (-jnp.arange(half, dtype=jnp.float32) / half)
    ang = pos[:, None] * inv[None, :]
    cos, sin = jnp.cos(ang), jnp.sin(ang)
    x32 = x.astype(jnp.float32)
    x1, x2 = x32[..., :half], x32[..., half:]
    return jnp.concatenate([x1 * cos - x2 * sin, x1 * sin + x2 * cos], -1).astype(x.dtype)


def retention(q, k, v):
    b, h, s, d = q.shape
    L = CHUNK
    nc = s // L
    log_gamma = jnp.log(1.0 - 2.0 ** (-5.0 - jnp.arange(h, dtype=jnp.float32)))
    idx = jnp.arange(L, dtype=jnp.float32)
    diff = idx[:, None] - idx[None, :]
    inner_decay = jnp.where(diff >= 0, jnp.exp(diff[None] * log_gamma[:, None, None]), 0.0)
    qc = q.astype(jnp.float32).reshape(b, h, nc, L, d)
    kc = (k.astype(jnp.float32) * d ** -0.5).reshape(b, h, nc, L, d)
    vc = v.astype(jnp.float32).reshape(b, h, nc, L, d)
    scores = jnp.einsum('bhcld,bhcmd->bhclm', qc, kc) * inner_decay[None, :, None]
    inner = jnp.einsum('bhclm,bhcme->bhcle', scores, vc)
    kv_w = jnp.exp((L - 1 - idx)[None, :] * log_gamma[:, None])
    local = jnp.einsum('bhcmd,bhcme->bhcde', kc * kv_w[None, :, None, :, None], vc)
    chunk_decay = jnp.exp(L * log_gamma)[None, :, None, None]

    def step(state, loc):
        return chunk_decay * state + loc, state

    _, r_prev = lax.scan(step, jnp.zeros((b, h, d, d), jnp.float32), jnp.moveaxis(local, 2, 0))
    r_prev = jnp.moveaxis(r_prev, 0, 2)
    q_w = jnp.exp((idx + 1.0)[None, :] * log_gamma[:, None])
    cross = jnp.einsum('bhcld,bhcde->bhcle', qc, r_prev) * q_w[None, :, None, :, None]
    return (inner + cross).reshape(b, h, s, d)


def mlstm(q, k, v, ig, fg):
    b, h, s, d = q.shape
    L = CHUNK
    nc = s // L
    qc = q.astype(jnp.float32).reshape(b, h, nc, L, d)
    kc = (k.astype(jnp.float32) * d ** -0.5).reshape(b, h, nc, L, d)
    vc = v.astype(jnp.float32).reshape(b, h, nc, L, d)
    ic = ig.astype(jnp.float32).reshape(b, h, nc, L)
    lf = jax.nn.log_sigmoid(fg.astype(jnp.float32)).reshape(b, h, nc, L)
    bcum = jnp.cumsum(lf, axis=-1)
    g = bcum[..., -1]
    a = g[..., None] - bcum + ic
    amax = a.max(-1)
    wa = jnp.exp(a - amax[..., None])
    loc_c = jnp.einsum('bhcl,bhcld,bhcle->bhcde', wa, kc, vc)
    loc_n = jnp.einsum('bhcl,bhcld->bhcd', wa, kc)

    def step(carry, xs):
        c_s, n_s, m_s = carry
        g_c, amax_c, lc, ln = xs
        m_new = jnp.maximum(g_c + m_s, amax_c)
        sp = jnp.exp(g_c + m_s - m_new)
        sl = jnp.exp(amax_c - m_new)
        c_new = sp[..., None, None] * c_s + sl[..., None, None] * lc
        n_new = sp[..., None] * n_s + sl[..., None] * ln
        return (c_new, n_new, m_new), (c_s, n_s, m_s)

    init = (jnp.zeros((b, h, d, d), jnp.float32), jnp.zeros((b, h, d), jnp.float32), jnp.full((b, h), NEG, jnp.float32))
    xs = (jnp.moveaxis(g, 2, 0), jnp.moveaxis(amax, 2, 0), jnp.moveaxis(loc_c, 2, 0), jnp.moveaxis(loc_n, 2, 0))
    _, (c_prev, n_prev, m_prev) = lax.scan(step, init, xs)
    c_prev = jnp.moveaxis(c_prev, 0, 2)
    n_prev = jnp.moveaxis(n_prev, 0, 2)
    m_prev = jnp.moveaxis(m_prev, 0, 2)
    idx = jnp.arange(L)
    causal = idx[:, None] >= idx[None, :]
    log_d = jnp.where(causal, bcum[..., :, None] - bcum[..., None, :] + ic[..., None, :], NEG)
    inter = bcum + m_prev[..., None]
    m_t = jnp.maximum(inter, log_d.max(-1))
    dw = jnp.exp(log_d - m_t[..., None])
    cmat = jnp.einsum('bhcld,bhcmd->bhclm', qc, kc) * dw
    si = jnp.exp(inter - m_t)
    num = jnp.einsum('bhclm,bhcme->bhcle', cmat, vc) + si[..., None] * jnp.einsum('bhcld,bhcde->bhcle', qc, c_prev)
    den = cmat.sum(-1) + si * jnp.einsum('bhcld,bhcd->bhcl', qc, n_prev)
    hcell = num / jnp.maximum(jnp.abs(den), jnp.exp(-m_t))[..., None]
    return hcell.reshape(b, h, s, d)


def mla_attention(q_nope, q_rope, k_nope, k_rope, v):
    b, h, s, dn = q_nope.shape
    dr = q_rope.shape[-1]
    nq = s // Q_BLOCK
    scale = (dn + dr) ** -0.5
    kpos = jnp.arange(s)
    qn_b = q_nope.reshape(b, h, nq, Q_BLOCK, dn).transpose(2, 0, 1, 3, 4)
    qr_b = q_rope.reshape(b, h, nq, Q_BLOCK, dr).transpose(2, 0, 1, 3, 4)

    def block(args):
        qn, qr, i = args
        sc = jnp.einsum('bhqd,bhkd->bhqk', qn, k_nope) + jnp.einsum('bhqr,bkr->bhqk', qr, k_rope)
        sc = sc.astype(jnp.float32) * scale
        qpos = i * Q_BLOCK + jnp.arange(Q_BLOCK)
        limit = (qpos // CHUNK + 1) * CHUNK
        sc = jnp.where(kpos[None, :] < limit[:, None], sc, NEG)
        p = jax.nn.softmax(sc, axis=-1).astype(v.dtype)
        return jnp.einsum('bhqk,bhkd->bhqd', p, v)

    out = lax.map(block, (qn_b, qr_b, jnp.arange(nq)))
    return out.transpose(1, 2, 0, 3, 4).reshape(b, h, s, v.shape[-1])


def causal_dwconv(x, w, bias):
    kw, c = w.shape
    y = lax.conv_general_dilated(x, w.reshape(kw, 1, c).astype(x.dtype), window_strides=(1,), padding=[(kw - 1, 0)], dimension_numbers=('NWC', 'WIO', 'NWC'), feature_group_count=c)
    return y + bias


def hybrid_mixer(x, pos, w_in, ret_gn_w, mla_q_norm_w, mla_w_uq, mla_kv_norm_w, mla_w_ukv, conv_w, conv_b, b_i, b_f, mlstm_gn_w, w_out):
    bsz, s, _ = x.shape
    hcat = x @ w_in
    offsets = np.cumsum(IN_SIZES)[:-1].tolist()
    (r_q, r_k, r_v, r_g, c_q, c_kv, k_r, m_q, m_k, m_v, m_o, m_i, m_f) = jnp.split(hcat, offsets, axis=-1)
    rq = rope(to_heads(r_q, RET_HEADS), pos)
    rk = rope(to_heads(r_k, RET_HEADS), pos)
    ret = retention(rq, rk, to_heads(r_v, RET_HEADS))
    ret_out = jax.nn.silu(r_g.astype(jnp.float32)) * head_norm(from_heads(ret), ret_gn_w, RET_HEADS)
    qf = (rms_norm(c_q, mla_q_norm_w) @ mla_w_uq).reshape(bsz, s, MLA_HEADS, MLA_NOPE + MLA_ROPE).transpose(0, 2, 1, 3)
    q_nope, q_rope = qf[..., :MLA_NOPE], rope(qf[..., MLA_NOPE:], pos)
    kvf = (rms_norm(c_kv, mla_kv_norm_w) @ mla_w_ukv).reshape(bsz, s, MLA_HEADS, MLA_NOPE + MLA_V).transpose(0, 2, 1, 3)
    k_nope, v_mla = kvf[..., :MLA_NOPE], kvf[..., MLA_NOPE:]
    k_rope = rope(k_r, pos)
    att_out = from_heads(mla_attention(q_nope, q_rope, k_nope, k_rope, v_mla))
    qk = jax.nn.silu(causal_dwconv(jnp.concatenate([m_q, m_k], -1), conv_w, conv_b))
    mq, mk = qk[..., :MLSTM_W], qk[..., MLSTM_W:]
    ig = (m_i + b_i).transpose(0, 2, 1)
    fg = (m_f + b_f).transpose(0, 2, 1)
    hcell = mlstm(to_heads(mq, MLSTM_HEADS), to_heads(mk, MLSTM_HEADS), to_heads(m_v, MLSTM_HEADS), ig, fg)
    h_gated = jax.nn.sigmoid(m_o.astype(jnp.float32)) * from_heads(hcell)
    mlstm_out = head_norm(h_gated, mlstm_gn_w, MLSTM_HEADS)
    merged = jnp.concatenate([ret_out.astype(x.dtype), att_out.astype(x.dtype), mlstm_out.astype(x.dtype)], -1)
    return merged @ w_out


def swiglu(x, wg, wu, wd):
    return (jax.nn.silu(x @ wg) * (x @ wu)) @ wd


def moe_swiglu(x, router, wg, wu, wd):
    bsz, s, d = x.shape
    t = x.reshape(bsz * s, d)
    logits = (t @ router).astype(jnp.float32)
    top_v, top_i = lax.top_k(logits, TOP_K)
    top_w = jax.nn.softmax(top_v, axis=-1)
    gate = jnp.sum(jax.nn.one_hot(top_i, N_EXPERTS, dtype=jnp.float32) * top_w[..., None], axis=1)
    y = jnp.zeros((bsz * s, d), jnp.float32)
    for e in range(N_EXPERTS):
        y = y + gate[:, e:e + 1] * swiglu(t, wg[e], wu[e], wd[e]).astype(jnp.float32)
    return y.astype(x.dtype).reshape(bsz, s, d)


def setup_inputs(seed: int = 0) -> dict:
    key = jax.random.key(seed)
    ks = jax.random.split(key, 24)
    L = DEPTH

    def nrm(k, shape, scale):
        return jax.random.normal(k, shape, jnp.float32) * scale

    col_scale = np.concatenate([np.full(n, DN_BETA if isv else 1.0, np.float32) for n, isv in zip(IN_SIZES, IN_IS_VALUE)])
    ukv_scale = np.tile(np.concatenate([np.ones(MLA_NOPE, np.float32), np.full(MLA_V, DN_BETA, np.float32)]), MLA_HEADS)
    return {
        'x': nrm(ks[0], (BATCH, SEQ, D_MODEL), 1.0),
        'w_in': nrm(ks[1], (L, D_MODEL, D_IN), D_MODEL ** -0.5) * jnp.asarray(col_scale),
        'ret_gn_w': 1.0 + nrm(ks[2], (L, RET_W), 0.01),
        'mla_q_norm_w': 1.0 + nrm(ks[3], (L, MLA_Q_RANK), 0.01),
        'mla_w_uq': nrm(ks[4], (L, MLA_Q_RANK, MLA_HEADS * (MLA_NOPE + MLA_ROPE)), MLA_Q_RANK ** -0.5),
        'mla_kv_norm_w': 1.0 + nrm(ks[5], (L, MLA_KV_RANK), 0.01),
        'mla_w_ukv': nrm(ks[6], (L, MLA_KV_RANK, MLA_HEADS * (MLA_NOPE + MLA_V)), MLA_KV_RANK ** -0.5) * jnp.asarray(ukv_scale),
        'mlstm_conv_w': nrm(ks[7], (L, MLSTM_CONV, 2 * MLSTM_W), MLSTM_CONV ** -0.5),
        'mlstm_conv_b': nrm(ks[8], (L, 2 * MLSTM_W), 0.01),
        'mlstm_b_i': nrm(ks[9], (L, MLSTM_HEADS), 0.1),
        'mlstm_b_f': jnp.linspace(3.0, 6.0, MLSTM_HEADS, dtype=jnp.float32)[None, :] + nrm(ks[10], (L, MLSTM_HEADS), 0.01),
        'mlstm_gn_w': 1.0 + nrm(ks[11], (L, MLSTM_W), 0.01),
        'w_out': nrm(ks[12], (L, MIX_W, D_MODEL), MIX_W ** -0.5 * DN_BETA),
        'ln1_w': 1.0 + nrm(ks[13], (L, D_MODEL), 0.01),
        'ln1_b': nrm(ks[14], (L, D_MODEL), 0.01),
        'ffn_w_gate': nrm(ks[15], (N_DENSE, D_MODEL, D_FF), D_MODEL ** -0.5),
        'ffn_w_up': nrm(ks[16], (N_DENSE, D_MODEL, D_FF), D_MODEL ** -0.5 * DN_BETA),
        'ffn_w_down': nrm(ks[17], (N_DENSE, D_FF, D_MODEL), D_FF ** -0.5 * DN_BETA),
        'moe_router': nrm(ks[18], (N_MOE, D_MODEL, N_EXPERTS), D_MODEL ** -0.5),
        'moe_w_gate': nrm(ks[19], (N_MOE, N_EXPERTS, D_MODEL, D_FF_EXPERT), D_MODEL ** -0.5),
        'moe_w_up': nrm(ks[20], (N_MOE, N_EXPERTS, D_MODEL, D_FF_EXPERT), D_MODEL ** -0.5 * DN_BETA),
        'moe_w_down': nrm(ks[21], (N_MOE, N_EXPERTS, D_FF_EXPERT, D_MODEL), D_FF_EXPERT ** -0.5 * DN_BETA),
        'ln2_w': 1.0 + nrm(ks[22], (L, D_MODEL), 0.01),
        'ln2_b': nrm(ks[23], (L, D_MODEL), 0.01),
    }


def reference(x, w_in, ret_gn_w, mla_q_norm_w, mla_w_uq, mla_kv_norm_w, mla_w_ukv, mlstm_conv_w, mlstm_conv_b, mlstm_b_i, mlstm_b_f, mlstm_gn_w, w_out, ln1_w, ln1_b, ffn_w_gate, ffn_w_up, ffn_w_down, moe_router, moe_w_gate, moe_w_up, moe_w_down, ln2_w, ln2_b):
    pos = jnp.arange(x.shape[1], dtype=jnp.float32)
    for l in range(DEPTH):
        mix = hybrid_mixer(x, pos, w_in[l], ret_gn_w[l], mla_q_norm_w[l], mla_w_uq[l], mla_kv_norm_w[l], mla_w_ukv[l], mlstm_conv_w[l], mlstm_conv_b[l], mlstm_b_i[l], mlstm_b_f[l], mlstm_gn_w[l], w_out[l])
        x = layer_norm(DN_ALPHA * x + mix, ln1_w[l], ln1_b[l])
        if l % 2 == 0:
            f = swiglu(x, ffn_w_gate[l // 2], ffn_w_up[l // 2], ffn_w_down[l // 2])
        else:
            f = moe_swiglu(x, moe_router[l // 2], moe_w_gate[l // 2], moe_w_up[l // 2], moe_w_down[l // 2])
        x = layer_norm(DN_ALPHA * x + f, ln2_w[l], ln2_b[l])
    return x
```

```python
import contextlib
import numpy as np
import concourse.bass as bass
import concourse.mybir as mybir
from concourse.bass_utils import run_bass_kernel_spmd

F32 = mybir.dt.float32
BF16 = mybir.dt.bfloat16
AF = mybir.ActivationFunctionType
ALU = mybir.AluOpType
AX = mybir.AxisListType


class _Op:
    __slots__ = ("eng", "fn", "deps", "is_dma", "dkey", "dcount", "sig", "sigidx", "idx")


class Prog:
    ENGS = ("pe", "act", "dve", "pool", "sp")

    def __init__(self, nc, same_engine_sync=True):
        self.nc = nc
        self.ops = []
        self.last_w = {}
        self.readers = {}
        self.dma_counts = {}
        self.same_engine_sync = same_engine_sync
        self.last_on = {}
        self.dma_since = []
        self.pending_bar = {}

    def barrier(self):
        b = set(self.last_on.values()) | set(self.dma_since)
        self.dma_since = []
        for e in self.ENGS:
            self.pending_bar[e] = set(self.pending_bar.get(e, ())) | b

    def op(self, eng, fn, reads=(), writes=(), dma=None):
        o = _Op()
        o.eng, o.fn, o.idx = eng, fn, len(self.ops)
        o.is_dma = dma is not None
        o.dkey = dma
        o.sig = False
        o.sigidx = 0
        deps = set(self.pending_bar.pop(eng, ()))
        if o.is_dma:
            self.dma_since.append(o.idx)
        else:
            self.last_on[eng] = o.idx
        for k in reads:
            w = self.last_w.get(k)
            if w is not None:
                deps.add(w)
        for k in writes:
            w = self.last_w.get(k)
            if w is not None:
                deps.add(w)
            for r in self.readers.get(k, ()):
                deps.add(r)
        deps.discard(o.idx)
        o.deps = deps
        if o.is_dma:
            self.dma_counts[dma] = self.dma_counts.get(dma, 0) + 1
            o.dcount = self.dma_counts[dma]
        for k in reads:
            self.readers.setdefault(k, []).append(o.idx)
        for k in writes:
            self.last_w[k] = o.idx
            self.readers[k] = []
        self.ops.append(o)
        return o

    def pe(self, fn, reads=(), writes=()):
        return self.op("pe", fn, reads, writes)

    def act(self, fn, reads=(), writes=()):
        return self.op("act", fn, reads, writes)

    def dve(self, fn, reads=(), writes=()):
        return self.op("dve", fn, reads, writes)

    def pool(self, fn, reads=(), writes=()):
        return self.op("pool", fn, reads, writes)

    def dma(self, queue, key, fn, reads=(), writes=()):
        return self.op(queue, fn, reads, writes, dma=key)

    def emit(self, final_wait_engine="sp"):
        nc = self.nc
        import os as _os
        lim = int(_os.environ.get("MK_LIMIT", "0"))
        if lim:
            self.ops = self.ops[:lim]
            self.dma_counts = {}
            for o in self.ops:
                if o.is_dma:
                    self.dma_counts[o.dkey] = max(self.dma_counts.get(o.dkey, 0), o.dcount)
        ops = self.ops
        needed = {}
        for o in ops:
            for d in o.deps:
                a = ops[d]
                if a.is_dma:
                    continue
                if a.eng == o.eng and not o.is_dma:
                    if a.eng == "pe" or not self.same_engine_sync:
                        continue
                a.sig = True
        cnt = {e: 0 for e in self.ENGS}
        for o in ops:
            if o.sig and not o.is_dma:
                cnt[o.eng] += 1
                o.sigidx = cnt[o.eng]
        with contextlib.ExitStack() as st:
            esem = {e: st.enter_context(nc.semaphore("s_" + e)) for e in self.ENGS}
            dsem = {k: st.enter_context(nc.semaphore("d_%d" % i)) for i, k in enumerate(self.dma_counts)}
            block = st.enter_context(nc.Block())
            per_eng = {e: [o for o in ops if o.eng == e] for e in self.ENGS}

            def run(e, engine):
                known = {}
                for o in per_eng[e]:
                    waits = {}
                    for d in o.deps:
                        a = ops[d]
                        if a.is_dma:
                            s, v = dsem[a.dkey], 16 * a.dcount
                        else:
                            if not a.sig:
                                continue
                            if a.eng == o.eng and not o.is_dma and (a.eng == "pe" or not self.same_engine_sync):
                                continue
                            s, v = esem[a.eng], a.sigidx
                        if waits.get(s, 0) < v:
                            waits[s] = v
                    for s, v in waits.items():
                        if known.get(s, 0) >= v:
                            continue
                        engine.wait_ge(s, v)
                        known[s] = v
                    ins = o.fn(engine)
                    if o.is_dma:
                        ins.then_inc(dsem[o.dkey], 16)
                    elif o.sig:
                        ins.then_inc(esem[e], 1)
                if e == final_wait_engine:
                    for k, c in self.dma_counts.items():
                        engine.wait_ge(dsem[k], 16 * c)

            @block.tensor
            def _(eng):
                run("pe", eng)

            @block.scalar
            def _(eng):
                run("act", eng)

            @block.vector
            def _(eng):
                run("dve", eng)

            @block.gpsimd
            def _(eng):
                run("pool", eng)

            @block.sync
            def _(eng):
                run("sp", eng)


S, D, NT = 2048, 1024, 16
DEPTH = 2
ALPHA = float((2 * DEPTH) ** 0.25)
EPS = 1e-5
D_IN = 2472
D_FF, D_FFE, NE = 2816, 3584, 8
LN8 = float(np.log(0.125))
SCALE_MLA = float(96 ** -0.5)

W_NAMES = ["w_in", "ret_gn_w", "mla_q_norm_w", "mla_w_uq", "mla_kv_norm_w", "mla_w_ukv", "mlstm_conv_w",
           "mlstm_conv_b", "mlstm_b_i", "mlstm_b_f", "mlstm_gn_w", "w_out", "ln1_w", "ln1_b", "ffn_w_gate",
           "ffn_w_up", "ffn_w_down", "moe_router", "moe_w_gate", "moe_w_up", "moe_w_down", "ln2_w", "ln2_b"]
W_SHAPES = {
    "w_in": [2, 1024, 2472], "ret_gn_w": [2, 256], "mla_q_norm_w": [2, 256], "mla_w_uq": [2, 256, 768],
    "mla_kv_norm_w": [2, 128], "mla_w_ukv": [2, 128, 1024], "mlstm_conv_w": [2, 4, 512], "mlstm_conv_b": [2, 512],
    "mlstm_b_i": [2, 4], "mlstm_b_f": [2, 4], "mlstm_gn_w": [2, 256], "w_out": [2, 1024, 1024], "ln1_w": [2, 1024],
    "ln1_b": [2, 1024], "ffn_w_gate": [1, 1024, 2816], "ffn_w_up": [1, 1024, 2816], "ffn_w_down": [1, 2816, 1024],
    "moe_router": [1, 1024, 8], "moe_w_gate": [1, 8, 1024, 3584], "moe_w_up": [1, 8, 1024, 3584],
    "moe_w_down": [1, 8, 3584, 1024], "ln2_w": [2, 1024], "ln2_b": [2, 1024]}


def host_consts():
    c = {}
    c["c_ident"] = np.eye(128, dtype=np.float32)
    j = np.arange(128)
    c["c_tri"] = (j[:, None] <= j[None, :]).astype(np.float32)
    m = np.ones((128, 128), np.float32)
    m[64:, :64] = 0.0
    c["c_mlamask"] = m
    t = np.arange(S, dtype=np.float64)
    inv = 10000.0 ** (-np.arange(32, dtype=np.float64) / 32)
    ang = t[None, :] * inv[:, None]
    cosr = np.concatenate([np.cos(ang), np.cos(ang)], 0)
    sinr = np.concatenate([-np.sin(ang), np.sin(ang)], 0)
    c["c_rc64"] = np.concatenate([cosr, cosr], 0).astype(np.float32)
    c["c_rs64"] = np.concatenate([sinr, sinr], 0).astype(np.float32)
    inv = 10000.0 ** (-np.arange(16, dtype=np.float64) / 16)
    ang = t[None, :] * inv[:, None]
    c["c_rc32"] = np.concatenate([np.cos(ang), np.cos(ang)], 0).astype(np.float32)
    c["c_rs32"] = np.concatenate([-np.sin(ang), np.sin(ang)], 0).astype(np.float32)
    lg = -np.log(1.0 - 2.0 ** (-5.0 - np.arange(4, dtype=np.float64)))
    c["c_retsp"] = np.tile(lg[None, :], (128, 1)).astype(np.float32)
    return c


class _Stop(Exception):
    pass


class MK:
    def __init__(self, phases=("mixer", "ffn"), layers=(0, 1), dbg=(), mix_parts=("ret", "mlstm", "mla")):
        self.phases, self.layers, self.dbg, self.mix_parts = phases, layers, dbg, mix_parts
        nc = self.nc = bass.Bass("TRN2", target_bir_lowering=False)
        self.P = Prog(nc)
        self.x_d = nc.dram_tensor("x", [S, D], F32, kind="ExternalInput").ap()
        self.out_d = nc.dram_tensor("out", [S, D], F32, kind="ExternalOutput").ap()
        self.w = {n: nc.dram_tensor(n, W_SHAPES[n], F32, kind="ExternalInput").ap() for n in W_NAMES}
        self.c = {n: nc.dram_tensor(n, list(v.shape), F32, kind="ExternalInput").ap() for n, v in host_consts().items()}
        self.dbg_d = {}
        self.uid = 0

    def sb(self, st, name, shape, dt):
        self.uid += 1
        return st.enter_context(self.nc.sbuf_tensor("%s_%d" % (name, self.uid), shape, dt))

    def ps(self, st, name, shape, dt):
        self.uid += 1
        return st.enter_context(self.nc.psum_tensor("%s_%d" % (name, self.uid), shape, dt))

    def dbg_out(self, name, shape):
        d = self.nc.dram_tensor("dbg_" + name, shape, F32, kind="ExternalOutput").ap()
        self.dbg_d[name] = d
        return d

    def build(self):
        nc, P = self.nc, self.P
        with contextlib.ExitStack() as st:
            self.xres = self.sb(st, "xres", [128, NT, D], F32)
            self.xT = self.sb(st, "xT", [128, 8, S], BF16)
            self.ident32 = self.sb(st, "ident32", [128, 128], F32)
            self.identb = self.sb(st, "identb", [128, 128], BF16)
            self.tri32 = self.sb(st, "tri32", [128, 128], F32)
            self.trib = self.sb(st, "trib", [128, 128], BF16)
            self.ones32 = self.sb(st, "ones32", [128, 128], F32)
            self.gate = self.sb(st, "gate", [128, NT, NE], F32)
            P.dma("sp", "c0", lambda e: e.dma_start(out=self.ident32[:], in_=self.c["c_ident"]), writes=["ident32"])
            P.dma("sp", "c1", lambda e: e.dma_start(out=self.tri32[:], in_=self.c["c_tri"]), writes=["tri32"])
            P.dma("pool", "c2", lambda e: e.dma_start(out=self.identb[:], in_=self.c["c_ident"]), writes=["identb"])
            P.dma("pool", "c3", lambda e: e.dma_start(out=self.trib[:], in_=self.c["c_tri"]), writes=["trib"])
            P.pool(lambda e: e.memset(self.ones32[:], 1.0), writes=["ones32"])
            self.epsc = self.sb(st, "epsc", [128, 1], F32)
            P.pool(lambda e: e.memset(self.epsc[:], EPS), writes=["epsc"])
            xv = self.x_d.rearrange("(t p) d -> p t d", p=128)
            for g in range(4):
                P.dma("sp", ("xl", g), lambda e, g=g: e.dma_start(out=self.xres[:, 4 * g:4 * g + 4, :], in_=xv[:, 4 * g:4 * g + 4, :]),
                      writes=[("xres", t) for t in range(4 * g, 4 * g + 4)])
            with contextlib.ExitStack() as s2:
                self.finish_setup(s2)
                self.skew([lambda t: self.finish_a(t, self.xres[:, t, :], [("xres", t)]), self.finish_b], NT)
            P.barrier()
            self.stopped = False
            for l in self.layers:
                if "mixer" in self.phases:
                    self.mixer(l)
                    if self.stopped:
                        break
                self.ln_phase(l, 1, router=(l % 2 == 1), last=False)
                if "ffn" in self.phases:
                    self.ffn(l)
                self.ln_phase(l, 2, router=False, last=(l == self.layers[-1]))
            P.emit()
        return nc

    def finish_setup(self, st, nxb=3):
        self.f_xb = [self.sb(st, "f_xb", [128, D], BF16) for _ in range(nxb)]
        self.f_pt = [self.ps(st, "f_pt", [128, 8, 128], BF16) for _ in range(2)]

    def finish_a(self, t, src, src_keys):
        P = self.P
        b = t % len(self.f_xb)
        xb = self.f_xb[b]
        P.act(lambda e: e.copy(out=xb[:], in_=src), reads=src_keys, writes=[("f_xb", b)])
        P.act(lambda e: e.activation(out=self.xres[:, t, :], in_=src, func=AF.Copy, scale=ALPHA),
              reads=src_keys, writes=[("xres", t)])

    def finish_b(self, t):
        P = self.P
        b = t % len(self.f_xb)
        pb = t % 2
        xb, pt = self.f_xb[b], self.f_pt[pb]
        for k in range(8):
            P.pe(lambda e, k=k: e.transpose(out=pt[:, k, :], in_=xb[:, k * 128:(k + 1) * 128], identity=self.identb[:]),
                 reads=[("f_xb", b), "identb"], writes=[("f_pt", pb)])
        P.dve(lambda e: e.tensor_copy(out=self.xT[:, :, t * 128:(t + 1) * 128], in_=pt[:]),
              reads=[("f_pt", pb)], writes=[("xT", t)])

    def finish_tile(self, t, src, src_keys):
        self.finish_a(t, src, src_keys)
        self.finish_b(t)

    @staticmethod
    def skew(stages, n):
        for step in range(n + len(stages) - 1):
            for sidx in range(len(stages) - 1, -1, -1):
                i = step - sidx
                if 0 <= i < n:
                    stages[sidx](i)

    def ln_phase(self, l, which, router, last):
        P, nc = self.P, self.nc
        with contextlib.ExitStack() as st:
            wt = self.sb(st, "ln_w", [128, D], F32)
            bt = self.sb(st, "ln_b", [128, D], F32)
            wn = "ln%d_w" % which
            bn = "ln%d_b" % which
            P.dma("sp", ("lnw", l, which), lambda e: e.dma_start(out=wt[:], in_=self.w[wn][l].partition_broadcast(128)), writes=["ln_w"])
            P.dma("sp", ("lnb", l, which), lambda e: e.dma_start(out=bt[:], in_=self.w[bn][l].partition_broadcast(128)), writes=["ln_b"])
            self.finish_setup(st)
            NS, NM, NX = 3, 4, 4
            stats = [self.sb(st, "ln_st", [128, 2, 6], F32) for _ in range(NS)]
            mv = [self.sb(st, "ln_mv", [128, 4], F32) for _ in range(NM)]
            xn = [self.sb(st, "ln_xn", [128, D], F32) for _ in range(NX)]
            if router:
                rw = self.sb(st, "rw", [128, 8, NE], F32)
                P.dma("sp", ("rw", l), lambda e: e.dma_start(out=rw[:], in_=self.w["moe_router"][l // 2].rearrange("(k p) n -> p k n", p=128)), writes=["rw"])
                xt32 = [self.sb(st, "xt32", [128, 8, 128], F32) for _ in range(2)]
                pt32 = [self.ps(st, "pt32", [128, 4, 128], F32) for _ in range(2)]
                plog = [self.ps(st, "plog", [128, NE], F32) for _ in range(2)]
                rs = [self.sb(st, "rs", [128, 40], F32) for _ in range(2)]

            def a0(t):
                sta, m = stats[t % NS], mv[t % NM]
                y = self.xres[:, t, :]
                for h in range(2):
                    P.dve(lambda e, h=h: e.bn_stats(out=sta[:, h, :], in_=y[:, h * 512:(h + 1) * 512]),
                          reads=[("xres", t)], writes=[("ln_st", t % NS)])
                P.dve(lambda e: e.bn_aggr(out=m[:, 0:2], in_=sta[:].rearrange("p a b -> p (a b)")), reads=[("ln_st", t % NS)], writes=[("ln_mv", t % NM)])

            def a1(t):
                m = mv[t % NM]
                km = [("ln_mv", t % NM)]
                P.act(lambda e: e.activation(out=m[:, 2:3], in_=m[:, 1:2], func=AF.Ln, bias=self.epsc[:], scale=1.0), reads=km + ["epsc"], writes=km)
                P.act(lambda e: e.activation(out=m[:, 2:3], in_=m[:, 2:3], func=AF.Exp, scale=-0.5), reads=km, writes=km)
                P.dve(lambda e: e.scalar_tensor_tensor(out=m[:, 3:4], in0=m[:, 0:1], scalar=-1.0, in1=m[:, 2:3], op0=ALU.mult, op1=ALU.mult), reads=km, writes=km)

            def a2(t):
                m, x_n = mv[t % NM], xn[t % NX]
                kx = [("ln_xn", t % NX)]
                P.act(lambda e: e.activation(out=x_n[:], in_=self.xres[:, t, :], func=AF.Identity, bias=m[:, 3:4], scale=m[:, 2:3]),
                      reads=[("xres", t), ("ln_mv", t % NM)], writes=kx)
                P.dve(lambda e: e.tensor_tensor(out=x_n[:], in0=x_n[:], in1=wt[:], op=ALU.mult), reads=kx + ["ln_w"], writes=kx)
                P.dve(lambda e: e.tensor_tensor(out=x_n[:], in0=x_n[:], in1=bt[:], op=ALU.add), reads=kx + ["ln_b"], writes=kx)

            def a3(t):
                x_n = xn[t % NX]
                kx = [("ln_xn", t % NX)]
                if last:
                    P.dma("sp", ("out", t), lambda e: e.dma_start(out=self.out_d[t * 128:(t + 1) * 128, :], in_=x_n[:]), reads=kx)
                    return
                self.finish_a(t, x_n[:], kx)
                if router:
                    b = t % 2
                    self.router_tile(t, x_n, rw, xt32[b], pt32, plog[b], rs[b], b, xkey=kx[0])

            def a4(t):
                if not last:
                    self.finish_b(t)

            self.skew([a0, a1, a2, a3, a4], NT)
        P.barrier()

    def router_tile(self, t, x_n, rw, xt32, pt32, plog, rs, b, xkey=None):
        P = self.P
        xkey = xkey or ("ln_xn", b)
        for hh in range(2):
            for k in range(4):
                kk = hh * 4 + k
                P.pe(lambda e, k=k, kk=kk, hh=hh: e.transpose(out=pt32[hh][:, k, :], in_=x_n[:, kk * 128:(kk + 1) * 128], identity=self.ident32[:]),
                     reads=[xkey, "ident32"], writes=[("pt32", hh)])
            P.act(lambda e, hh=hh: e.copy(out=xt32[:, hh * 4:hh * 4 + 4, :], in_=pt32[hh][:]), reads=[("pt32", hh)], writes=[("xt32", b)])
        for k in range(8):
            P.pe(lambda e, k=k: e.matmul(plog[:], lhsT=xt32[:, k, :], rhs=rw[:, k, :], start=(k == 0), stop=(k == 7)),
                 reads=[("xt32", b), "rw"], writes=[("plog", b)])
        K = [("rs", b)]
        lg = rs[:, 32:40]
        P.act(lambda e: e.copy(out=lg, in_=plog[:]), reads=[("plog", b)], writes=K)
        P.dve(lambda e: e.reduce_max(out=rs[:, 0:1], in_=lg, axis=AX.X), reads=K, writes=K)
        P.dve(lambda e: e.tensor_scalar(out=rs[:, 8:16], in0=lg, scalar1=rs[:, 0:1], scalar2=None, op0=ALU.is_equal), reads=K, writes=K)
        P.dve(lambda e: e.scalar_tensor_tensor(out=rs[:, 16:24], in0=rs[:, 8:16], scalar=-1e30, in1=lg, op0=ALU.mult, op1=ALU.add), reads=K, writes=K)
        P.dve(lambda e: e.reduce_max(out=rs[:, 1:2], in_=rs[:, 16:24], axis=AX.X), reads=K, writes=K)
        P.dve(lambda e: e.tensor_scalar(out=rs[:, 24:32], in0=rs[:, 16:24], scalar1=rs[:, 1:2], scalar2=None, op0=ALU.is_equal), reads=K, writes=K)
        P.dve(lambda e: e.tensor_tensor(out=rs[:, 2:3], in0=rs[:, 1:2], in1=rs[:, 0:1], op=ALU.subtract), reads=K, writes=K)
        P.act(lambda e: e.activation(out=rs[:, 3:4], in_=rs[:, 2:3], func=AF.Exp), reads=K, writes=K)
        P.dve(lambda e: e.tensor_scalar(out=rs[:, 4:5], in0=rs[:, 3:4], scalar1=1.0, scalar2=None, op0=ALU.add), reads=K, writes=K)
        P.dve(lambda e: e.reciprocal(out=rs[:, 4:5], in_=rs[:, 4:5]), reads=K, writes=K)
        P.dve(lambda e: e.tensor_tensor(out=rs[:, 5:6], in0=rs[:, 3:4], in1=rs[:, 4:5], op=ALU.mult), reads=K, writes=K)
        P.dve(lambda e: e.tensor_scalar(out=self.gate[:, t, :], in0=rs[:, 8:16], scalar1=rs[:, 4:5], scalar2=None, op0=ALU.mult),
              reads=K, writes=[("gate", t)])
        P.dve(lambda e: e.scalar_tensor_tensor(out=self.gate[:, t, :], in0=rs[:, 24:32], scalar=rs[:, 5:6], in1=self.gate[:, t, :], op0=ALU.mult, op1=ALU.add),
              reads=K + [("gate", t)], writes=[("gate", t)])

    def ffn(self, l):
        P = self.P
        if l % 2 == 0:
            F = D_FF
            experts = [(self.w["ffn_w_gate"][l // 2], self.w["ffn_w_up"][l // 2], self.w["ffn_w_down"][l // 2], None)]
        else:
            F = D_FFE
            experts = [(self.w["moe_w_gate"][l // 2][e], self.w["moe_w_up"][l // 2][e], self.w["moe_w_down"][l // 2][e], e) for e in range(NE)]
        nchunks = F // 128
        with contextlib.ExitStack() as st:
            NB = 2
            wgb = [self.sb(st, "wgb", [128, 8, 512], BF16) for _ in range(NB)]
            wub = [self.sb(st, "wub", [128, 8, 512], BF16) for _ in range(NB)]
            wdb = [self.sb(st, "wdb", [128, 4, D], BF16) for _ in range(NB)]
            hT = [self.sb(st, "hT", [128, 4, S], BF16) for _ in range(2)]
            sg = [self.sb(st, "sg", [128, 512], F32) for _ in range(2)]
            pg = [self.ps(st, "pg", [128, 512], F32) for _ in range(2)]
            pu = [self.ps(st, "pu", [128, 512], F32) for _ in range(2)]
            pd = [self.ps(st, "pd", [128, 512], F32) for _ in range(4)]
            gi = 0
            xi = 0
            yi = 0
            for (wg, wu, wd, ei) in experts:
                wgv = wg.rearrange("(k p) n -> p k n", p=128)
                wuv = wu.rearrange("(k p) n -> p k n", p=128)
                wdv = wd.rearrange("(c p) n -> p c n", p=128)
                c0 = 0
                while c0 < nchunks:
                    nch = min(4, nchunks - c0)
                    b = gi % NB
                    hb = gi % 2
                    P.dma("pool", ("wg", b), lambda e, b=b, c0=c0, nch=nch, wgv=wgv: e.dma_start(out=wgb[b][:, :, 0:nch * 128], in_=wgv[:, :, c0 * 128:(c0 + nch) * 128]),
                          writes=[("wgb", b)])
                    P.dma("pool", ("wu", b), lambda e, b=b, c0=c0, nch=nch, wuv=wuv: e.dma_start(out=wub[b][:, :, 0:nch * 128], in_=wuv[:, :, c0 * 128:(c0 + nch) * 128]),
                          writes=[("wub", b)])
                    P.dma("pool", ("wd", b), lambda e, b=b, c0=c0, nch=nch, wdv=wdv: e.dma_start(out=wdb[b][:, 0:nch, :], in_=wdv[:, c0:c0 + nch, :]),
                          writes=[("wdb", b)])
                    for j in range(nch):
                        for t in range(4):
                            x = xi % 2
                            xi += 1
                            xk = [("xT", tt) for tt in range(4 * t, 4 * t + 4)]
                            for k in range(8):
                                P.pe(lambda e, k=k, j=j, t=t, x=x, b=b: e.matmul(pg[x][:], lhsT=wgb[b][:, k, j * 128:(j + 1) * 128], rhs=self.xT[:, k, t * 512:(t + 1) * 512], start=(k == 0), stop=(k == 7)),
                                     reads=[("wgb", b)] + xk, writes=[("pg", x)])
                            for k in range(8):
                                P.pe(lambda e, k=k, j=j, t=t, x=x, b=b: e.matmul(pu[x][:], lhsT=wub[b][:, k, j * 128:(j + 1) * 128], rhs=self.xT[:, k, t * 512:(t + 1) * 512], start=(k == 0), stop=(k == 7)),
                                     reads=[("wub", b)] + xk, writes=[("pu", x)])
                            P.act(lambda e, x=x: e.activation(out=sg[x][:], in_=pg[x][:], func=AF.Silu), reads=[("pg", x)], writes=[("sg", x)])
                            P.dve(lambda e, x=x, j=j, t=t, hb=hb: e.tensor_tensor(out=hT[hb][:, j, t * 512:(t + 1) * 512], in0=sg[x][:], in1=pu[x][:], op=ALU.mult),
                                  reads=[("sg", x), ("pu", x)], writes=[("hT", hb, j, t)])
                    for tile in range(NT):
                        for half in range(2):
                            y = yi % 4
                            yi += 1
                            for j in range(nch):
                                P.pe(lambda e, j=j, tile=tile, half=half, y=y, b=b, hb=hb, nch=nch: e.matmul(pd[y][:], lhsT=hT[hb][:, j, tile * 128:(tile + 1) * 128], rhs=wdb[b][:, j, half * 512:(half + 1) * 512], start=(j == 0), stop=(j == nch - 1)),
                                     reads=[("hT", hb, j, tile // 4), ("wdb", b)], writes=[("pd", y)])
                            xr = self.xres[:, tile, half * 512:(half + 1) * 512]
                            if ei is None:
                                P.dve(lambda e, y=y, xr=xr: e.tensor_tensor(out=xr, in0=xr, in1=pd[y][:], op=ALU.add),
                                      reads=[("pd", y), ("xres", tile)], writes=[("xres", tile)])
                            else:
                                P.dve(lambda e, y=y, xr=xr, tile=tile, ei=ei: e.scalar_tensor_tensor(out=xr, in0=pd[y][:], scalar=self.gate[:, tile, ei:ei + 1], in1=xr, op0=ALU.mult, op1=ALU.add),
                                      reads=[("pd", y), ("xres", tile), ("gate", tile)], writes=[("xres", tile)])
                    c0 += nch
                    gi += 1
        P.barrier()


def make_in_maps(inputs, cores):
    consts = host_consts()
    shared = {n: np.ascontiguousarray(np.asarray(inputs[n], dtype=np.float32)) for n in W_NAMES}
    maps = []
    for c in cores:
        m = {"x": np.ascontiguousarray(np.asarray(inputs["x"][c], dtype=np.float32))}
        m.update(shared)
        m.update(consts)
        maps.append(m)
    return maps


def kernel(**inputs):
    mk = MK()
    nc = mk.build()
    res = run_bass_kernel_spmd(nc, make_in_maps(inputs, list(range(8))), core_ids=list(range(8)))
    return np.stack([np.asarray(r["out"], dtype=np.float32) for r in res.results], 0)


def _mixer(self, l):
    with contextlib.ExitStack() as st:
        self.mergedT = self.sb(st, "mergedT", [128, 8, S], BF16)
        if "dbg_merged" in self.dbg:
            self.P.pool(lambda e: e.memset(self.mergedT[:], 0.0), writes=[("mT", c, t) for c in range(8) for t in range(NT)])
        if "ret" in self.mix_parts:
            self.phase_ret(l)
        if "mlstm" in self.mix_parts:
            self.phase_mlstm(l)
        if "mla" in self.mix_parts:
            self.phase_mla(l)
        if "dbg_merged" in self.dbg:
            d = self.dbg_out("merged%d" % l, [128, 8, S])
            with contextlib.ExitStack() as s2:
                t32 = self.sb(s2, "dbgm", [128, 8, S // 4], F32)
                for q in range(4):
                    self.P.act(lambda e, q=q: e.copy(out=t32[:], in_=self.mergedT[:, :, q * 512:(q + 1) * 512]),
                               reads=[("mT", c, t) for c in range(8) for t in range(NT)], writes=["dbgm"])
                    self.P.dma("sp", ("dbgm", q), lambda e, q=q: e.dma_start(out=d[:, :, q * 512:(q + 1) * 512], in_=t32[:]), reads=["dbgm"])
            self.P.barrier()
            if "stop" in self.dbg:
                self.stopped = True
                return
        self.phase_wout(l)


def _phase_wout(self, l):
    P = self.P
    with contextlib.ExitStack() as st:
        wo = self.sb(st, "wo", [128, 8, D], BF16)
        P.dma("pool", ("wo", l), lambda e: e.dma_start(out=wo[:], in_=self.w["w_out"][l].rearrange("(k p) n -> p k n", p=128)), writes=["wo"])
        pw = [self.ps(st, "pw", [128, 512], F32) for _ in range(4)]
        yi = 0
        for tile in range(NT):
            for half in range(2):
                y = yi % 4
                yi += 1
                for k in range(8):
                    P.pe(lambda e, k=k, tile=tile, half=half, y=y: e.matmul(pw[y][:], lhsT=self.mergedT[:, k, tile * 128:(tile + 1) * 128], rhs=wo[:, k, half * 512:(half + 1) * 512], start=(k == 0), stop=(k == 7)),
                         reads=["wo"] + [("mT", c, tile) for c in range(8)], writes=[("pw", y)])
                xr = self.xres[:, tile, half * 512:(half + 1) * 512]
                P.dve(lambda e, y=y, xr=xr: e.tensor_tensor(out=xr, in0=xr, in1=pw[y][:], op=ALU.add),
                      reads=[("pw", y), ("xres", tile)], writes=[("xres", tile)])
    P.barrier()


def _phase_ret(self, l):
    P = self.P
    with contextlib.ExitStack() as st:
        wR = self.sb(st, "wR", [128, 8, 1024], BF16)
        P.dma("pool", ("wR", l), lambda e: e.dma_start(out=wR[:], in_=self.w["w_in"][l][:, 0:1024].rearrange("(k p) n -> p k n", p=128)), writes=["wR"])
        q32 = self.sb(st, "q32", [128, 2, S], BF16)
        k32 = self.sb(st, "k32", [128, 2, S], BF16)
        with contextlib.ExitStack() as s2:
            wRsw = self.sb(s2, "wRsw", [128, 8, 512], BF16)
            v = wR[:, :, 0:512].rearrange("p k (h t j) -> p k h t j", t=2, j=32)
            vs = wRsw[:].rearrange("p k (h t j) -> p k h t j", t=2, j=32)
            P.act(lambda e: e.copy(out=vs[:, :, :, 0, :], in_=v[:, :, :, 1, :]), reads=["wR"], writes=["wRsw"])
            P.act(lambda e: e.copy(out=vs[:, :, :, 1, :], in_=v[:, :, :, 0, :]), reads=["wR"], writes=["wRsw"])
            rc = [self.sb(s2, "rc", [128, 512], F32) for _ in range(2)]
            rs = [self.sb(s2, "rs", [128, 512], F32) for _ in range(2)]
            tmp = [self.sb(s2, "rtmp", [128, 512], F32) for _ in range(2)]
            tmp2 = [self.sb(s2, "rtmp2", [128, 512], F32) for _ in range(2)]
            pa = [self.ps(s2, "pa", [128, 512], F32) for _ in range(2)]
            pb = [self.ps(s2, "pb", [128, 512], F32) for _ in range(2)]
            xi = 0
            for t in range(4):
                tb = t % 2
                P.dma("sp", ("rc", tb), lambda e, t=t, tb=tb: e.dma_start(out=rc[tb][:], in_=self.c["c_rc64"][:, t * 512:(t + 1) * 512]), writes=[("rc", tb)])
                P.dma("sp", ("rs", tb), lambda e, t=t, tb=tb: e.dma_start(out=rs[tb][:], in_=self.c["c_rs64"][:, t * 512:(t + 1) * 512]), writes=[("rs", tb)])
                xk = [("xT", tt) for tt in range(4 * t, 4 * t + 4)]
                for c in range(4):
                    x = xi % 2
                    xi += 1
                    for k in range(8):
                        P.pe(lambda e, k=k, c=c, t=t, x=x: e.matmul(pa[x][:], lhsT=wR[:, k, c * 128:(c + 1) * 128], rhs=self.xT[:, k, t * 512:(t + 1) * 512], start=(k == 0), stop=(k == 7)),
                             reads=["wR"] + xk, writes=[("pa", x)])
                    for k in range(8):
                        P.pe(lambda e, k=k, c=c, t=t, x=x: e.matmul(pb[x][:], lhsT=wRsw[:, k, c * 128:(c + 1) * 128], rhs=self.xT[:, k, t * 512:(t + 1) * 512], start=(k == 0), stop=(k == 7)),
                             reads=["wRsw"] + xk, writes=[("pb", x)])
                    dst = (q32 if c < 2 else k32)[:, c % 2, t * 512:(t + 1) * 512]
                    dk = ("qk32", c, t)
                    P.dve(lambda e, x=x, tb=tb: e.tensor_tensor(out=tmp[x][:], in0=pa[x][:], in1=rc[tb][:], op=ALU.mult),
                          reads=[("pa", x), ("rc", tb)], writes=[("rtmp", x)])
                    P.dve(lambda e, x=x, tb=tb: e.tensor_tensor(out=tmp2[x][:], in0=pb[x][:], in1=rs[tb][:], op=ALU.mult),
                          reads=[("pb", x), ("rs", tb)], writes=[("rtmp2", x)])
                    P.dve(lambda e, x=x, dst=dst: e.tensor_tensor(out=dst, in0=tmp2[x][:], in1=tmp[x][:], op=ALU.add),
                          reads=[("rtmp2", x), ("rtmp", x)], writes=[dk])
        P.barrier()
        self.linattn(st, l, "ret", q32, k32, lambda k: wR[:, k, 512:1024], None, 0, self.w["ret_gn_w"][l])
    P.barrier()


MK.mixer = _mixer
MK.phase_wout = _phase_wout
MK.phase_ret = _phase_ret


def _linattn(self, st, l, kind, q32, k32, wtok, wgate, chunk0, gnw_ap, bibf=None):
    P = self.P
    ml = kind == "mlstm"

    class Ring:
        def __init__(ring, name, shp, dt, n):
            ring.name, ring.n = name, n
            ring.t = [self.sb(st, kind + name, shp, dt) for _ in range(n)]

        def __call__(ring, i):
            return ring.t[i % ring.n]

        def k(ring, i):
            return (kind, ring.name, i % ring.n)

    vaug = Ring("vaug", [128, 4, 65], BF16, 6)
    sgt = Ring("sgt", [128, 256], F32, 6)
    gcp = Ring("gcp", [128, 256], F32, 1 if ml else 6)
    gt = Ring("gt", [128, 8], F32, 3)
    spt = Ring("spt", [128, 4], F32, 3)
    ut = Ring("ut", [128, 4], F32, 2)
    sprep = Ring("sprep", [128, 4, 64], F32, 2)
    urep = Ring("urep", [128, 4, 64], F32, 2)
    eBU = Ring("eBU", [128, 4, 128], F32, 2)
    eg = Ring("eg", [128, 4], F32, 5)
    qs = Ring("qs", [128, 2, 2, 128], BF16, 3)
    ks = Ring("ks", [128, 2, 128], BF16, 2)
    ktok = Ring("ktok", [128, 256], BF16, 2)
    cm = Ring("cm", [128, 4, 128], BF16, 2)
    yb = Ring("y", [128, 4, 64], F32, 2)
    ysqr = Ring("ysq", [128, 4, 64], F32, 2)
    s8 = Ring("s8", [128, 20], F32, 2)
    zb = Ring("zb", [128, 256], BF16, 2)
    sbf = lambda n, shp, dt: self.sb(st, kind + n, shp, dt)
    S32 = sbf("S32", [128, 2, 65], F32)
    Sb = sbf("Sb", [128, 2, 65], BF16)
    stmp = sbf("stmp", [128, 2, 65], F32)
    gnw = sbf("gnw", [128, 256], F32)
    cst = sbf("cst", [128, 2], F32)
    pv = self.ps(st, "pv", [128, 512], F32)
    pBU = self.ps(st, "pBU", [128, 4, 128], F32)
    psc = self.ps(st, "psc", [128, 4, 128], F32)
    psm = self.ps(st, "psm", [128, 16], F32)
    pob = self.ps(st, "pob", [128, 260], F32)
    pub = self.ps(st, "pub", [128, 260], F32)
    pbk = self.ps(st, "pbk", [128, 256], BF16)
    pbz = self.ps(st, "pbz", [128, 256], BF16)
    pB, pG, pgt = psm[:, 0:4], psm[:, 4:8], psm[:, 8:16]
    po = pob[:].rearrange("p (h e) -> p h e", e=65)
    pu = pub[:].rearrange("p (c r e) -> p c r e", r=2, e=65)
    ptk = pbk[:].rearrange("p (c m) -> p c m", m=128)
    ptz = pbz[:].rearrange("p (c m) -> p c m", m=128)
    K = lambda n: (kind, n)

    P.dma("sp", (kind, "gnw", l), lambda e: e.dma_start(out=gnw[:], in_=gnw_ap.partition_broadcast(128)), writes=[K("gnw")])
    P.pool(lambda e: e.memset(cst[:, 0:1], LN8), writes=[K("cst")])
    P.pool(lambda e: e.memset(cst[:, 1:2], 1.0), writes=[K("cst")])
    P.pool(lambda e: e.memset(S32[:], 0.0), writes=[K("S32")])
    P.pool(lambda e: e.memset(Sb[:], 0.0), writes=[K("Sb")])
    for b in range(vaug.n):
        P.pool(lambda e, b=b: e.memset(vaug.t[b][:], 1.0), writes=[vaug.k(b)])
    for b in range(qs.n):
        P.pool(lambda e, b=b: e.memset(qs.t[b][:], 0.0), writes=[qs.k(b)])
    if ml:
        bibf_t = sbf("bibf", [128, 8], F32)
        P.dma("sp", (kind, "bi", l), lambda e: e.dma_start(out=bibf_t[:, 0:4], in_=bibf[0].partition_broadcast(128)), writes=[K("bibf")])
        P.dma("sp", (kind, "bf", l), lambda e: e.dma_start(out=bibf_t[:, 4:8], in_=bibf[1].partition_broadcast(128)), writes=[K("bibf")])
    else:
        for b in range(spt.n):
            P.dma("sp", (kind, "sp", b, l), lambda e, b=b: e.dma_start(out=spt.t[b][:], in_=self.c["c_retsp"]), writes=[spt.k(b)])
            P.pool(lambda e, b=b: e.memset(gt.t[b][:], 0.0), writes=[gt.k(b)])

    def t0(i):
        cols = slice(i * 128, (i + 1) * 128)
        xk = [("xT", i)]
        for k in range(8):
            P.pe(lambda e, k=k: e.matmul(pv[:], lhsT=self.xT[:, k, cols], rhs=wtok(k), start=(k == 0), stop=(k == 7)),
                 reads=xk, writes=[K("pv")])
        if ml:
            for k in range(8):
                P.pe(lambda e, k=k: e.matmul(pgt, lhsT=self.xT[:, k, cols], rhs=wgate(k), start=(k == 0), stop=(k == 7)),
                     reads=xk, writes=[K("psm")])
            P.dve(lambda e: e.tensor_tensor(out=gt(i)[:], in0=pgt, in1=bibf_t[:], op=ALU.add), reads=[K("psm"), K("bibf")], writes=[gt.k(i)])
        P.act(lambda e: e.copy(out=vaug(i)[:, :, 0:64], in_=pv[:, 0:256].rearrange("p (h d) -> p h d", d=64)),
              reads=[K("pv")], writes=[vaug.k(i)])
        P.act(lambda e: e.activation(out=sgt(i)[:], in_=pv[:, 256:512], func=AF.Exp, scale=-1.0), reads=[K("pv")], writes=[sgt.k(i)])
        if not ml:
            P.act(lambda e: e.copy(out=gcp(i)[:], in_=pv[:, 256:512]), reads=[K("pv")], writes=[gcp.k(i)])
        P.act(lambda e: e.activation(out=sgt(i)[:], in_=sgt(i)[:], func=AF.Ln, bias=cst[:, 1:2], scale=1.0), reads=[sgt.k(i), K("cst")], writes=[sgt.k(i)])
        P.act(lambda e: e.activation(out=sgt(i)[:], in_=sgt(i)[:], func=AF.Exp, scale=-1.0), reads=[sgt.k(i)], writes=[sgt.k(i)])
        if ml:
            P.act(lambda e: e.activation(out=spt(i)[:], in_=gt(i)[:, 4:8], func=AF.Exp, scale=-1.0), reads=[gt.k(i)], writes=[spt.k(i)])
            P.act(lambda e: e.activation(out=spt(i)[:], in_=spt(i)[:], func=AF.Ln, bias=cst[:, 1:2], scale=1.0), reads=[spt.k(i), K("cst")], writes=[spt.k(i)])

    def t1(i):
        P.pe(lambda e: e.matmul(pB, lhsT=self.tri32[:], rhs=spt(i)[:], start=True, stop=True), reads=["tri32", spt.k(i)], writes=[K("psm")])
        P.pe(lambda e: e.matmul(pG, lhsT=self.ones32[:], rhs=spt(i)[:], start=True, stop=True), reads=["ones32", spt.k(i)], writes=[K("psm")])
        P.dve(lambda e: e.tensor_tensor(out=ut(i)[:], in0=pB, in1=gt(i)[:, 0:4], op=ALU.add), reads=[K("psm"), gt.k(i)], writes=[ut.k(i)])
        P.act(lambda e: e.activation(out=eg(i)[:], in_=pG, func=AF.Exp, scale=-1.0), reads=[K("psm")], writes=[eg.k(i)])
        P.act(lambda e: e.copy(out=sprep(i)[:], in_=spt(i)[:].unsqueeze(2).broadcast_to([128, 4, 64])), reads=[spt.k(i)], writes=[sprep.k(i)])
        P.act(lambda e: e.copy(out=urep(i)[:], in_=ut(i)[:].unsqueeze(2).broadcast_to([128, 4, 64])), reads=[ut.k(i)], writes=[urep.k(i)])

    def t2(i):
        cols = slice(i * 128, (i + 1) * 128)
        for c in range(2):
            P.pe(lambda e, c=c: e.matmul(pBU[:, c, :], lhsT=sprep(i)[:, 2 * c:2 * c + 2, :].rearrange("p a b -> p (a b)"), rhs=self.tri32[:], start=True, stop=True),
                 reads=[sprep.k(i), "tri32"], writes=[K("pBU")])
        for c in range(2):
            P.pe(lambda e, c=c: e.matmul(pBU[:, 2 + c, :], lhsT=urep(i)[:, 2 * c:2 * c + 2, :].rearrange("p a b -> p (a b)"), rhs=self.ident32[:], start=True, stop=True),
                 reads=[urep.k(i), "ident32"], writes=[K("pBU")])
        P.act(lambda e: e.activation(out=eBU(i)[:, 0:2, :], in_=pBU[:, 0:2, :], func=AF.Exp, scale=-1.0), reads=[K("pBU")], writes=[eBU.k(i)])
        P.act(lambda e: e.activation(out=eBU(i)[:, 2:4, :], in_=pBU[:, 2:4, :], func=AF.Exp, bias=cst[:, 0:1], scale=1.0), reads=[K("pBU"), K("cst")], writes=[eBU.k(i)])
        qk_keys = [("qk32", c, i // 4) for c in range(4)]
        for r in range(2):
            rr = slice(r * 64, (r + 1) * 64)
            P.dve(lambda e, r=r, rr=rr: e.tensor_tensor(out=qs(i)[rr, :, r, :], in0=q32[rr, :, cols], in1=eBU(i)[rr, 0:2, :], op=ALU.mult), reads=qk_keys + [eBU.k(i)], writes=[qs.k(i)])
        P.dve(lambda e: e.tensor_tensor(out=ks(i)[:], in0=k32[:, :, cols], in1=eBU(i)[:, 2:4, :], op=ALU.mult), reads=qk_keys + [eBU.k(i)], writes=[ks.k(i)])

    def t3(i):
        for c in range(2):
            P.pe(lambda e, c=c: e.transpose(out=ptk[:, c, :], in_=ks(i)[:, c, :], identity=self.identb[:]), reads=[ks.k(i), "identb"], writes=[K("pbk")])
        for h in range(4):
            c, r = h // 2, h % 2
            P.pe(lambda e, h=h, c=c, r=r: e.matmul(psc[:, h, :], lhsT=ks(i)[:, c, :], rhs=qs(i)[:, c, r, :], start=True, stop=True),
                 reads=[ks.k(i), qs.k(i)], writes=[K("psc")])
        P.act(lambda e: e.copy(out=ktok(i)[:].rearrange("p (c m) -> p c m", m=128), in_=ptk), reads=[K("pbk")], writes=[ktok.k(i)])
        P.dve(lambda e: e.tensor_tensor(out=cm(i)[:], in0=psc[:], in1=self.tri32[:].unsqueeze(1).broadcast_to([128, 4, 128]), op=ALU.mult),
              reads=[K("psc"), "tri32"], writes=[cm.k(i)])

    def t4(i):
        for h in range(4):
            c, r = h // 2, h % 2
            P.pe(lambda e, h=h: e.matmul(po[:, h, :], lhsT=cm(i)[:, h, :], rhs=vaug(i)[:, h, :], start=True, stop=False),
                 reads=[cm.k(i), vaug.k(i)], writes=[K("po")])
            P.pe(lambda e, h=h, c=c, r=r: e.matmul(po[:, h, :], lhsT=qs(i)[:, c, r, :], rhs=Sb[:, c, :], start=False, stop=True),
                 reads=[qs.k(i), K("Sb")], writes=[K("po")])
        for c in range(2):
            P.pe(lambda e, c=c: e.matmul(pu[:, c, :, :].rearrange("p r e -> p (r e)"), lhsT=ktok(i)[:, c * 128:(c + 1) * 128], rhs=vaug(i)[:, 2 * c:2 * c + 2, :].rearrange("p a e -> p (a e)"), start=True, stop=True),
                 reads=[ktok.k(i), vaug.k(i)], writes=[K("pu")])
        for r in range(2):
            rows = slice(r * 64, (r + 1) * 64)
            egb = eg(i)[rows, :].rearrange("p (c r) -> p c r", r=2)[:, :, r].unsqueeze(2).broadcast_to([64, 2, 65])
            P.dve(lambda e, r=r, rows=rows: e.tensor_tensor(out=stmp[rows], in0=S32[rows], in1=pu[rows, :, r, :], op=ALU.add),
                  reads=[K("S32"), K("pu")], writes=[K("stmp")])
            P.dve(lambda e, rows=rows, egb=egb: e.tensor_tensor(out=S32[rows], in0=stmp[rows], in1=egb, op=ALU.mult),
                  reads=[K("stmp"), eg.k(i)], writes=[K("S32")])
            P.act(lambda e, rows=rows: e.copy(out=Sb[rows], in_=S32[rows]), reads=[K("S32")], writes=[K("Sb")])
        y = yb(i)
        s = s8(i)
        KY, KS = [yb.k(i)], [s8.k(i)]
        if ml:
            P.act(lambda e: e.activation(out=s[:, 16:20], in_=po[:, :, 64], func=AF.Abs), reads=[K("po")], writes=KS)
        P.act(lambda e: e.copy(out=y[:], in_=po[:, :, 0:64]), reads=[K("po")], writes=KY)

    def t5(i):
        y = yb(i)
        s = s8(i)
        ysq = ysqr(i)
        KY, KS = [yb.k(i)], [s8.k(i)]
        if ml:
            P.dve(lambda e: e.tensor_scalar(out=s[:, 16:20], in0=s[:, 16:20], scalar1=1.0, scalar2=None, op0=ALU.max), reads=KS, writes=KS)
            P.dve(lambda e: e.reciprocal(out=s[:, 16:20], in_=s[:, 16:20]), reads=KS, writes=KS)
            P.dve(lambda e: e.tensor_tensor(out=y[:], in0=y[:], in1=s[:, 16:20].unsqueeze(2).broadcast_to([128, 4, 64]), op=ALU.mult), reads=KY + KS, writes=KY)
            P.dve(lambda e: e.tensor_tensor(out=y[:], in0=y[:], in1=sgt(i)[:].rearrange("p (h d) -> p h d", d=64), op=ALU.mult), reads=KY + [sgt.k(i)], writes=KY)
        P.act(lambda e: e.activation(out=ysq[:], in_=y[:], func=AF.Square), reads=KY, writes=[ysqr.k(i)])
        P.dve(lambda e: e.reduce_sum(out=s[:, 0:4], in_=y[:], axis=AX.X), reads=KY, writes=KS)
        P.dve(lambda e: e.reduce_sum(out=s[:, 4:8], in_=ysq[:], axis=AX.X), reads=[ysqr.k(i)], writes=KS)
        P.dve(lambda e: e.tensor_scalar(out=s[:, 0:4], in0=s[:, 0:4], scalar1=1.0 / 64, scalar2=None, op0=ALU.mult), reads=KS, writes=KS)
        P.dve(lambda e: e.tensor_tensor(out=s[:, 8:12], in0=s[:, 0:4], in1=s[:, 0:4], op=ALU.mult), reads=KS, writes=KS)
        P.dve(lambda e: e.scalar_tensor_tensor(out=s[:, 4:8], in0=s[:, 4:8], scalar=1.0 / 64, in1=s[:, 8:12], op0=ALU.mult, op1=ALU.subtract), reads=KS, writes=KS)
        P.act(lambda e: e.activation(out=s[:, 12:16], in_=s[:, 4:8], func=AF.Ln, bias=self.epsc[:], scale=1.0), reads=KS + ["epsc"], writes=KS)
        P.act(lambda e: e.activation(out=s[:, 12:16], in_=s[:, 12:16], func=AF.Exp, scale=-0.5), reads=KS, writes=KS)
        P.dve(lambda e: e.tensor_tensor(out=y[:], in0=y[:], in1=s[:, 0:4].unsqueeze(2).broadcast_to([128, 4, 64]), op=ALU.subtract), reads=KY + KS, writes=KY)
        P.dve(lambda e: e.tensor_tensor(out=y[:], in0=y[:], in1=s[:, 12:16].unsqueeze(2).broadcast_to([128, 4, 64]), op=ALU.mult), reads=KY + KS, writes=KY)
        yf = y[:].rearrange("p h d -> p (h d)")
        if ml:
            P.dve(lambda e: e.tensor_tensor(out=zb(i)[:], in0=yf, in1=gnw[:], op=ALU.mult), reads=KY + [K("gnw")], writes=[zb.k(i)])
        else:
            P.dve(lambda e: e.tensor_tensor(out=yf, in0=yf, in1=gnw[:], op=ALU.mult), reads=KY + [K("gnw")], writes=KY)
            P.dve(lambda e: e.tensor_tensor(out=yf, in0=yf, in1=gcp(i)[:], op=ALU.mult), reads=KY + [gcp.k(i)], writes=KY)
            P.dve(lambda e: e.tensor_tensor(out=zb(i)[:], in0=yf, in1=sgt(i)[:], op=ALU.mult), reads=KY + [sgt.k(i)], writes=[zb.k(i)])

    def t6(i):
        cols = slice(i * 128, (i + 1) * 128)
        for c in range(2):
            P.pe(lambda e, c=c: e.transpose(out=ptz[:, c, :], in_=zb(i)[:, c * 128:(c + 1) * 128], identity=self.identb[:]), reads=[zb.k(i), "identb"], writes=[K("pbz")])
        P.act(lambda e: e.copy(out=self.mergedT[:, chunk0:chunk0 + 2, cols], in_=ptz), reads=[K("pbz")],
              writes=[("mT", chunk0, i), ("mT", chunk0 + 1, i)])

    stages = [t0, t1, t2, t3, t4, t5, t6]
    for step in range(NT + len(stages) - 1):
        for sidx in range(len(stages) - 1, -1, -1):
            i = step - sidx
            if 0 <= i < NT:
                stages[sidx](i)


MK.linattn = _linattn


def _phase_mlstm(self, l):
    P = self.P
    with contextlib.ExitStack() as st:
        wM = self.sb(st, "wM", [128, 8, 1032], BF16)
        P.dma("pool", ("wM", l), lambda e: e.dma_start(out=wM[:], in_=self.w["w_in"][l][:, 1440:2472].rearrange("(k p) n -> p k n", p=128)), writes=["wM"])
        q32 = self.sb(st, "mq32", [128, 2, S], BF16)
        k32 = self.sb(st, "mk32", [128, 2, S], BF16)
        with contextlib.ExitStack() as s2:
            cw = self.sb(s2, "cw", [128, 4, 4], F32)
            cb = self.sb(s2, "cb", [128, 4], F32)
            for j in range(4):
                P.dma("sp", ("cw", j, l), lambda e, j=j: e.dma_start(out=cw[:, :, j], in_=self.w["mlstm_conv_w"][l][j].rearrange("(c p) -> p c", p=128), allow_slow_non_contiguous=True), writes=["cw"])
            P.dma("sp", ("cb", l), lambda e: e.dma_start(out=cb[:], in_=self.w["mlstm_conv_b"][l].rearrange("(c p) -> p c", p=128), allow_slow_non_contiguous=True), writes=["cb"])
            pre = [self.sb(s2, "pre", [128, 3 + S], F32) for _ in range(2)]
            accs = [self.sb(s2, "cacc", [128, S], F32) for _ in range(2)]
            pa = [self.ps(s2, "mpa", [128, 512], F32) for _ in range(2)]
            for b in range(2):
                P.pool(lambda e, b=b: e.memset(pre[b][:, 0:3], 0.0), writes=[("pre", b)])
            xi = 0
            for c in range(4):
                pb = c % 2
                for t in range(4):
                    x = xi % 2
                    xi += 1
                    xk = [("xT", tt) for tt in range(4 * t, 4 * t + 4)]
                    for k in range(8):
                        P.pe(lambda e, k=k, c=c, t=t, x=x: e.matmul(pa[x][:], lhsT=wM[:, k, c * 128:(c + 1) * 128], rhs=self.xT[:, k, t * 512:(t + 1) * 512], start=(k == 0), stop=(k == 7)),
                             reads=["wM"] + xk, writes=[("mpa", x)])
                    P.act(lambda e, x=x, t=t, pb=pb: e.copy(out=pre[pb][:, 3 + t * 512:3 + (t + 1) * 512], in_=pa[x][:]), reads=[("mpa", x)], writes=[("pre", pb)])
                dst = (q32 if c < 2 else k32)[:, c % 2, :]
                dk = [("qk32", c, t) for t in range(4)]
                acc = accs[pb]
                ak = [("cacc", pb)]
                P.dve(lambda e, c=c, pb=pb, acc=acc: e.tensor_scalar(out=acc[:], in0=pre[pb][:, 0:S], scalar1=cw[:, c, 0:1], scalar2=None, op0=ALU.mult),
                      reads=[("pre", pb), "cw"], writes=ak)
                for j in range(1, 4):
                    P.dve(lambda e, c=c, j=j, pb=pb, acc=acc: e.scalar_tensor_tensor(out=acc[:], in0=pre[pb][:, j:j + S], scalar=cw[:, c, j:j + 1], in1=acc[:], op0=ALU.mult, op1=ALU.add),
                          reads=[("pre", pb), "cw"] + ak, writes=ak)
                P.act(lambda e, c=c, dst=dst, acc=acc: e.activation(out=dst, in_=acc[:], func=AF.Silu, bias=cb[:, c:c + 1], scale=1.0), reads=ak + ["cb"], writes=dk)
        P.barrier()
        self.linattn(st, l, "mlstm", q32, k32, lambda k: wM[:, k, 512:1024], lambda k: wM[:, k, 1024:1032], 6,
                     self.w["mlstm_gn_w"][l], (self.w["mlstm_b_i"][l], self.w["mlstm_b_f"][l]))
    P.barrier()


MK.phase_mlstm = _phase_mlstm


def _phase_mla(self, l):
    P = self.P
    with contextlib.ExitStack() as st:
        wuq = self.sb(st, "wuq", [128, 2, 768], BF16)
        wukv = self.sb(st, "wukv", [128, 1024], BF16)
        wq_pad = self.sb(st, "wq_pad", [128, 2, 8, 128], BF16)
        wq_sw = self.sb(st, "wq_sw", [128, 2, 8, 32], BF16)
        wk_pad = self.sb(st, "wk_pad", [128, 8, 128], BF16)
        wv_pad = self.sb(st, "wv_pad", [128, 8, 64], BF16)
        nw = self.sb(st, "nw", [128, 4], F32)
        cqn = self.sb(st, "cqn", [128, 2, S], BF16)
        ckvn = self.sb(st, "ckvn", [128, S], BF16)
        krope = self.sb(st, "krope", [32, S], BF16)
        P.dma("pool", ("wuq", l), lambda e: e.dma_start(out=wuq[:], in_=self.w["mla_w_uq"][l].rearrange("(k p) n -> p k n", p=128)), writes=["wuq"])
        P.dma("pool", ("wukv", l), lambda e: e.dma_start(out=wukv[:], in_=self.w["mla_w_ukv"][l]), writes=["wukv"])
        P.dma("sp", ("nwq", l), lambda e: e.dma_start(out=nw[:, 0:2], in_=self.w["mla_q_norm_w"][l].rearrange("(c p) -> p c", p=128), allow_slow_non_contiguous=True), writes=["nw"])
        P.dma("sp", ("nwk", l), lambda e: e.dma_start(out=nw[:, 2:3], in_=self.w["mla_kv_norm_w"][l].rearrange("(c p) -> p c", p=128), allow_slow_non_contiguous=True), writes=["nw"])
        uqv = wuq[:].rearrange("p k (h c) -> p k h c", c=96)
        P.pool(lambda e: e.memset(wq_pad[:], 0.0), writes=["wq_pad"])
        P.pool(lambda e: e.tensor_copy(out=wq_pad[:, :, :, 0:32], in_=uqv[:, :, :, 64:96]), reads=["wuq"], writes=["wq_pad"])
        P.pool(lambda e: e.tensor_copy(out=wq_pad[:, :, :, 64:128], in_=uqv[:, :, :, 0:64]), reads=["wuq"], writes=["wq_pad"])
        P.pool(lambda e: e.tensor_copy(out=wq_sw[:, :, :, 0:16], in_=uqv[:, :, :, 80:96]), reads=["wuq"], writes=["wq_sw"])
        P.pool(lambda e: e.tensor_copy(out=wq_sw[:, :, :, 16:32], in_=uqv[:, :, :, 64:80]), reads=["wuq"], writes=["wq_sw"])
        ukv = wukv[:].rearrange("p (h c) -> p h c", c=128)
        P.pool(lambda e: e.memset(wk_pad[:], 0.0), writes=["wk_pad"])
        P.pool(lambda e: e.tensor_copy(out=wk_pad[:, :, 64:128], in_=ukv[:, :, 0:64]), reads=["wukv"], writes=["wk_pad"])
        P.pool(lambda e: e.tensor_copy(out=wv_pad[:], in_=ukv[:, :, 64:128]), reads=["wukv"], writes=["wv_pad"])

        with contextlib.ExitStack() as s2:
            wA = self.sb(s2, "wA", [128, 8, 416], BF16)
            wAsw = self.sb(s2, "wAsw", [128, 8, 32], BF16)
            P.dma("pool", ("wA", l), lambda e: e.dma_start(out=wA[:], in_=self.w["w_in"][l][:, 1024:1440].rearrange("(k p) n -> p k n", p=128)), writes=["wA"])
            P.pool(lambda e: e.tensor_copy(out=wAsw[:, :, 0:16], in_=wA[:, :, 400:416]), reads=["wA"], writes=["wAsw"])
            P.pool(lambda e: e.tensor_copy(out=wAsw[:, :, 16:32], in_=wA[:, :, 384:400]), reads=["wA"], writes=["wAsw"])
            c32 = self.sb(s2, "c32", [128, 3, 512], F32)
            sq = self.sb(s2, "sq", [128, 3, 512], F32)
            rstd = self.sb(s2, "rstd", [128, 2, 512], F32)
            rc = self.sb(s2, "rc32", [32, 512], F32)
            rs = self.sb(s2, "rs32", [32, 512], F32)
            t1 = self.sb(s2, "kt1", [32, 512], F32)
            t2 = self.sb(s2, "kt2", [32, 512], F32)
            pc = [self.ps(s2, "pc", [128, 512], F32) for _ in range(3)]
            pkr = self.ps(s2, "pkr", [32, 512], F32)
            pks = self.ps(s2, "pks", [32, 512], F32)
            pss = [self.ps(s2, "pss", [128, 512], F32) for _ in range(2)]

            def a1(t):
                tg = slice(t * 512, (t + 1) * 512)
                xk = [("xT", tt) for tt in range(4 * t, 4 * t + 4)]
                P.dma("sp", ("rc32", l), lambda e: e.dma_start(out=rc[:], in_=self.c["c_rc32"][:, tg]), writes=["rc32"])
                P.dma("sp", ("rs32", l), lambda e: e.dma_start(out=rs[:], in_=self.c["c_rs32"][:, tg]), writes=["rs32"])
                for c in range(3):
                    for k in range(8):
                        P.pe(lambda e, c=c, k=k: e.matmul(pc[c][:], lhsT=wA[:, k, c * 128:(c + 1) * 128], rhs=self.xT[:, k, tg], start=(k == 0), stop=(k == 7)),
                             reads=["wA"] + xk, writes=[("pc", c)])
                for k in range(8):
                    P.pe(lambda e, k=k: e.matmul(pkr[:], lhsT=wA[:, k, 384:416], rhs=self.xT[:, k, tg], start=(k == 0), stop=(k == 7)), reads=["wA"] + xk, writes=["pkr"])
                for k in range(8):
                    P.pe(lambda e, k=k: e.matmul(pks[:], lhsT=wAsw[:, k, :], rhs=self.xT[:, k, tg], start=(k == 0), stop=(k == 7)), reads=["wAsw"] + xk, writes=["pks"])
                for c in range(3):
                    P.act(lambda e, c=c: e.copy(out=c32[:, c, :], in_=pc[c][:]), reads=[("pc", c)], writes=[("c32", c)])
                    P.act(lambda e, c=c: e.activation(out=sq[:, c, :], in_=pc[c][:], func=AF.Square), reads=[("pc", c)], writes=[("sq", c)])
                for c in range(2):
                    P.pe(lambda e, c=c: e.matmul(pss[0][:], lhsT=self.ones32[:], rhs=sq[:, c, :], start=(c == 0), stop=(c == 1)), reads=[("sq", c), "ones32"], writes=[("pss", 0)])
                P.pe(lambda e: e.matmul(pss[1][:], lhsT=self.ones32[:], rhs=sq[:, 2, :], start=True, stop=True), reads=[("sq", 2), "ones32"], writes=[("pss", 1)])
                for i, n in ((0, 256.0), (1, 128.0)):
                    P.act(lambda e, i=i, n=n: e.activation(out=rstd[:, i, :], in_=pss[i][:], func=AF.Ln, bias=self.epsc[:], scale=1.0 / n), reads=[("pss", i), "epsc"], writes=[("rstd", i)])
                    P.act(lambda e, i=i: e.activation(out=rstd[:, i, :], in_=rstd[:, i, :], func=AF.Exp, scale=-0.5), reads=[("rstd", i)], writes=[("rstd", i)])
                for c in range(2):
                    P.dve(lambda e, c=c: e.scalar_tensor_tensor(out=cqn[:, c, tg], in0=c32[:, c, :], scalar=nw[:, c:c + 1], in1=rstd[:, 0, :], op0=ALU.mult, op1=ALU.mult),
                          reads=[("c32", c), "nw", ("rstd", 0)], writes=[("cqn", t)])
                P.dve(lambda e: e.scalar_tensor_tensor(out=ckvn[:, tg], in0=c32[:, 2, :], scalar=nw[:, 2:3], in1=rstd[:, 1, :], op0=ALU.mult, op1=ALU.mult),
                      reads=[("c32", 2), "nw", ("rstd", 1)], writes=[("ckvn", t)])
                P.dve(lambda e: e.tensor_tensor(out=t1[:], in0=pkr[:], in1=rc[:], op=ALU.mult), reads=["pkr", "rc32"], writes=["kt1"])
                P.dve(lambda e: e.tensor_tensor(out=t2[:], in0=pks[:], in1=rs[:], op=ALU.mult), reads=["pks", "rs32"], writes=["kt2"])
                P.dve(lambda e: e.tensor_tensor(out=krope[:, tg], in0=t1[:], in1=t2[:], op=ALU.add), reads=["kt1", "kt2"], writes=[("krope", t)])

            for t in range(4):
                a1(t)
        P.barrier()

        KT0 = self.sb(st, "KT", [128, 2, S], BF16)
        QT0 = self.sb(st, "QT", [128, 2, S], BF16)
        Vaug0 = self.sb(st, "Vaug", [128, NT, 2, 128], BF16)
        sets = [dict(KT=KT0[:], QT=QT0[:], V=Vaug0[:]),
                dict(KT=self.xT[:, 0:2, :], QT=self.xT[:, 2:4, :],
                     V=self.xT[:, 4:6, :].rearrange("p a (t r d) -> p (a t) r d", r=2, d=128))]
        PT = [self.sb(st, "PT", [128, 512], BF16) for _ in range(2)]
        sqf = [self.sb(st, "sqf", [128, 512], F32) for _ in range(2)]
        dsh = [self.sb(st, "dsh", [128, 512], F32) for _ in range(2)]
        qt1 = self.sb(st, "qt1", [32, 512], F32)
        qt2 = self.sb(st, "qt2", [32, 512], F32)
        rq = self.sb(st, "rq", [64, 512], F32)
        kmxs = [self.sb(st, "kmx", [128, 8], F32) for _ in range(2)]
        mask = self.sb(st, "mlamask", [128, 128], BF16)
        rcf = self.sb(st, "rcf", [32, 512], F32)
        rsf = self.sb(st, "rsf", [32, 512], F32)
        P.dma("pool", ("mask", l), lambda e: e.dma_start(out=mask[:], in_=self.c["c_mlamask"]), writes=["mlamask"])
        for si in range(2):
            P.pool(lambda e, si=si: e.memset(sets[si]["V"], 1.0), writes=[("Vaug", si)])
        pk = [self.ps(st, "pk", [128, 512], F32) for _ in range(2)]
        pbd = self.ps(st, "pbd", [128, 512], F32)
        pqs = self.ps(st, "pqs", [32, 512], F32)
        pn = self.ps(st, "pn", [128, 512], F32)
        pvv = self.ps(st, "pvv", [128, 4, 128], F32)
        po = [self.ps(st, "pmo", [128, 512], F32) for _ in range(2)]
        cnt = {"sq": 0, "n": 0}

        def build(hp):
            si = hp % 2
            KT, QT, Vaug, kmx = sets[si]["KT"], sets[si]["QT"], sets[si]["V"], kmxs[si]
            KK = lambda r, t: ("KT", si, r, t)
            QQ = lambda r, t: ("QT", si, r, t)
            for r in range(2):
                h = 2 * hp + r
                for t in range(4):
                    tg = slice(t * 512, (t + 1) * 512)
                    x = cnt["sq"] % 2
                    cnt["sq"] += 1
                    kk = [KK(r, t)]
                    P.pe(lambda e, h=h, tg=tg: e.matmul(pbd[:], lhsT=wk_pad[:, h, :], rhs=ckvn[:, tg], start=True, stop=True), reads=["wk_pad", ("ckvn", t)], writes=["pbd"])
                    P.act(lambda e, r=r, tg=tg: e.copy(out=KT[:, r, tg], in_=pbd[:]), reads=["pbd"], writes=kk)
                    P.act(lambda e, r=r, tg=tg: e.copy(out=KT[0:32, r, tg], in_=krope[:, tg]), reads=[("krope", t)] + kk, writes=kk)
                    P.pool(lambda e, r=r, tg=tg: e.memset(KT[32:33, r, tg], 1.0), reads=kk, writes=kk)
                    P.act(lambda e, r=r, tg=tg, x=x: e.activation(out=sqf[x][:], in_=KT[:, r, tg], func=AF.Square), reads=kk, writes=[("sqf", x)])
                    P.pe(lambda e, x=x: e.matmul(pn[:], lhsT=self.ones32[:], rhs=sqf[x][:], start=True, stop=True), reads=[("sqf", x), "ones32"], writes=["pn"])
                    P.dve(lambda e, r=r, t=t: e.reduce_max(out=kmx[:, r * 4 + t:r * 4 + t + 1], in_=pn[:], axis=AX.X), reads=["pn"], writes=[("kmx", si)])
                    yield
            for r in range(2):
                P.dve(lambda e, r=r: e.reduce_max(out=kmx[:, r * 4:r * 4 + 1], in_=kmx[:, r * 4:r * 4 + 4], axis=AX.X), reads=[("kmx", si)], writes=[("kmx", si)])
            for g in range(4):
                for ii in range(4):
                    i = 4 * g + ii
                    P.pe(lambda e, i=i, ii=ii: e.matmul(pvv[:, ii, :], lhsT=ckvn[:, i * 128:(i + 1) * 128], rhs=wv_pad[:, 2 * hp:2 * hp + 2, :].rearrange("p a d -> p (a d)"), start=True, stop=True),
                         reads=["wv_pad", ("ckvn", g)], writes=["pvv"])
                P.act(lambda e, g=g: e.copy(out=Vaug[:, 4 * g:4 * g + 4, 0, 0:64], in_=pvv[:, :, 0:64]), reads=["pvv"], writes=[("Vaug", si)])
                P.dve(lambda e, g=g: e.tensor_copy(out=Vaug[:, 4 * g:4 * g + 4, 1, 64:128], in_=pvv[:, :, 64:128]), reads=["pvv"], writes=[("Vaug", si)])
                yield
            for r in range(2):
                h = 2 * hp + r
                for t in range(4):
                    tg = slice(t * 512, (t + 1) * 512)
                    x = cnt["sq"] % 2
                    cnt["sq"] += 1
                    for kc in range(2):
                        P.pe(lambda e, h=h, tg=tg, kc=kc: e.matmul(pbd[:], lhsT=wq_pad[:, kc, h, :], rhs=cqn[:, kc, tg], start=(kc == 0), stop=(kc == 1)), reads=["wq_pad", ("cqn", t)], writes=["pbd"])
                    for kc in range(2):
                        P.pe(lambda e, h=h, tg=tg, kc=kc: e.matmul(pqs[:], lhsT=wq_sw[:, kc, h, :], rhs=cqn[:, kc, tg], start=(kc == 0), stop=(kc == 1)), reads=["wq_sw", ("cqn", t)], writes=["pqs"])
                    QK = [QQ(r, t)]
                    P.dma("sp", "rcf", lambda e, tg=tg: e.dma_start(out=rcf[:], in_=self.c["c_rc32"][:, tg]), writes=["rcf"])
                    P.dma("sp", "rsf", lambda e, tg=tg: e.dma_start(out=rsf[:], in_=self.c["c_rs32"][:, tg]), writes=["rsf"])
                    P.act(lambda e, r=r, tg=tg: e.copy(out=QT[64:128, r, tg], in_=pbd[64:128, :]), reads=["pbd"], writes=QK)
                    P.act(lambda e, r=r, tg=tg: e.copy(out=QT[32:64, r, tg], in_=pbd[32:64, :]), reads=["pbd"], writes=QK)
                    P.dve(lambda e: e.tensor_tensor(out=qt1[:], in0=pbd[0:32, :], in1=rcf[:], op=ALU.mult), reads=["pbd", "rcf"], writes=["qt1"])
                    P.dve(lambda e: e.tensor_tensor(out=qt2[:], in0=pqs[:], in1=rsf[:], op=ALU.mult), reads=["pqs", "rsf"], writes=["qt2"])
                    P.dve(lambda e, r=r, tg=tg: e.tensor_tensor(out=QT[0:32, r, tg], in0=qt1[:], in1=qt2[:], op=ALU.add), reads=["qt1", "qt2"], writes=QK)
                    P.act(lambda e, r=r, tg=tg, x=x: e.activation(out=sqf[x][:], in_=QT[:, r, tg], func=AF.Square), reads=QK, writes=[("sqf", x)])
                    P.pe(lambda e, x=x: e.matmul(pn[:], lhsT=self.ones32[:], rhs=sqf[x][:], start=True, stop=True), reads=[("sqf", x), "ones32"], writes=["pn"])
                    P.dve(lambda e, r=r: e.tensor_scalar(out=rq[32:33, :], in0=pn[32:33, :], scalar1=kmx[32:33, r * 4:r * 4 + 1], scalar2=None, op0=ALU.mult), reads=["pn", ("kmx", si)], writes=["rq"])
                    P.act(lambda e: e.activation(out=rq[32:33, :], in_=rq[32:33, :], func=AF.Ln), reads=["rq"], writes=["rq"])
                    P.act(lambda e: e.activation(out=rq[32:33, :], in_=rq[32:33, :], func=AF.Exp, scale=0.5), reads=["rq"], writes=["rq"])
                    P.dve(lambda e, r=r, tg=tg: e.tensor_scalar(out=QT[32:33, r, tg], in0=rq[32:33, :], scalar1=-1.0, scalar2=None, op0=ALU.mult), reads=["rq"] + QK, writes=QK)
                    yield

        def attend(hp):
            si = hp % 2
            KT, QT, Vaug = sets[si]["KT"], sets[si]["QT"], sets[si]["V"]
            items = []
            for r in range(2):
                for g in range(4):
                    for j in range(4 * g + 4):
                        items.append((r, g, j))

            def qk(n):
                r, g, j = items[n]
                x = cnt["n"] % 2
                q_lo = max(512 * g, 128 * j)
                N = 512 * (g + 1) - q_lo
                P.pe(lambda e: e.matmul(pk[x][:, 0:N], lhsT=KT[:, r, j * 128:(j + 1) * 128], rhs=QT[:, r, q_lo:q_lo + N], start=True, stop=True),
                     reads=[("KT", si, r, j // 4)] + [("QT", si, r, tt) for tt in range(q_lo // 512, g + 1)], writes=[("pk", x)])
                P.act(lambda e: e.activation(out=PT[x][:, 0:N], in_=pk[x][:, 0:N], func=AF.Exp, scale=SCALE_MLA), reads=[("pk", x)], writes=[("PT", x)])
                if 128 * j >= 512 * g:
                    P.dve(lambda e: e.tensor_tensor(out=PT[x][:, 0:128], in0=PT[x][:, 0:128], in1=mask[:], op=ALU.mult), reads=[("PT", x), "mlamask"], writes=[("PT", x)])
                cnt["n"] += 1
                return x

            def pv(n, x):
                r, g, j = items[n]
                og = (r * 4 + g) % 2
                q_lo = max(512 * g, 128 * j)
                N = 512 * (g + 1) - q_lo
                off = q_lo - 512 * g
                last = (j == 4 * g + 3)
                P.pe(lambda e: e.matmul(po[og][:, off:off + N], lhsT=Vaug[:, j, r, :], rhs=PT[x][:, 0:N], start=(j == 0), stop=last),
                     reads=[("Vaug", si), ("PT", x)], writes=[("pmo", og)])
                if last:
                    nr = slice(r * 64, (r + 1) * 64)
                    dr = slice((1 - r) * 64, (2 - r) * 64)
                    P.act(lambda e: e.activation(out=dsh[og][nr, :], in_=po[og][dr, :], func=AF.Ln), reads=[("pmo", og)], writes=[("dsh", og)])
                    P.act(lambda e: e.activation(out=dsh[og][nr, :], in_=dsh[og][nr, :], func=AF.Exp, scale=-1.0), reads=[("dsh", og)], writes=[("dsh", og)])
                    P.dve(lambda e: e.tensor_tensor(out=self.mergedT[nr, 2 + hp, g * 512:(g + 1) * 512], in0=po[og][nr, :], in1=dsh[og][nr, :], op=ALU.mult),
                          reads=[("pmo", og), ("dsh", og)], writes=[("mT", 2 + hp, tt) for tt in range(4 * g, 4 * g + 4)])

            prev = None
            for n in range(len(items)):
                x = qk(n)
                if prev is not None:
                    pv(*prev)
                prev = (n, x)
                if n % 4 == 3:
                    yield
            pv(*prev)
            yield

        def drain(g):
            for _ in g:
                pass

        drain(build(0))
        for hp in range(4):
            ga = attend(hp)
            gb = build(hp + 1) if hp + 1 < 4 else iter(())
            a_done = b_done = False
            while not (a_done and b_done):
                if not b_done:
                    try:
                        next(gb)
                    except StopIteration:
                        b_done = True
                if not a_done:
                    try:
                        next(ga)
                    except StopIteration:
                        a_done = True
    P.barrier()


MK.phase_mla = _phase_mla
```

```python
import contextlib
import numpy as np
import concourse.bass as bass
import concourse.mybir as mybir
from concourse.bass_utils import run_bass_kernel_spmd

F32 = mybir.dt.float32
BF16 = mybir.dt.bfloat16
AF = mybir.ActivationFunctionType
ALU = mybir.AluOpType
AX = mybir.AxisListType


class _Op:
    __slots__ = ("eng", "fn", "deps", "is_dma", "dkey", "dcount", "sig", "sigidx", "idx")


class Prog:
    ENGS = ("pe", "act", "dve", "pool", "sp")

    def __init__(self, nc, same_engine_sync=True):
        self.nc = nc
        self.ops = []
        self.last_w = {}
        self.readers = {}
        self.dma_counts = {}
        self.same_engine_sync = same_engine_sync
        self.last_on = {}
        self.dma_since = []
        self.pending_bar = {}

    def barrier(self):
        b = set(self.last_on.values()) | set(self.dma_since)
        self.dma_since = []
        for e in self.ENGS:
            self.pending_bar[e] = set(self.pending_bar.get(e, ())) | b

    def op(self, eng, fn, reads=(), writes=(), dma=None):
        o = _Op()
        o.eng, o.fn, o.idx = eng, fn, len(self.ops)
        o.is_dma = dma is not None
        o.dkey = dma
        o.sig = False
        o.sigidx = 0
        deps = set(self.pending_bar.pop(eng, ()))
        if o.is_dma:
            self.dma_since.append(o.idx)
        else:
            self.last_on[eng] = o.idx
        for k in reads:
            w = self.last_w.get(k)
            if w is not None:
                deps.add(w)
        for k in writes:
            w = self.last_w.get(k)
            if w is not None:
                deps.add(w)
            for r in self.readers.get(k, ()):
                deps.add(r)
        deps.discard(o.idx)
        o.deps = deps
        if o.is_dma:
            self.dma_counts[dma] = self.dma_counts.get(dma, 0) + 1
            o.dcount = self.dma_counts[dma]
        for k in reads:
            self.readers.setdefault(k, []).append(o.idx)
        for k in writes:
            self.last_w[k] = o.idx
            self.readers[k] = []
        self.ops.append(o)
        return o

    def pe(self, fn, reads=(), writes=()):
        return self.op("pe", fn, reads, writes)

    def act(self, fn, reads=(), writes=()):
        return self.op("act", fn, reads, writes)

    def dve(self, fn, reads=(), writes=()):
        return self.op("dve", fn, reads, writes)

    def pool(self, fn, reads=(), writes=()):
        return self.op("pool", fn, reads, writes)

    def dma(self, queue, key, fn, reads=(), writes=()):
        return self.op(queue, fn, reads, writes, dma=key)

    def emit(self, final_wait_engine="sp"):
        nc = self.nc
        import os as _os
        lim = int(_os.environ.get("MK_LIMIT", "0"))
        if lim:
            self.ops = self.ops[:lim]
            self.dma_counts = {}
            for o in self.ops:
                if o.is_dma:
                    self.dma_counts[o.dkey] = max(self.dma_counts.get(o.dkey, 0), o.dcount)
        ops = self.ops
        needed = {}
        for o in ops:
            for d in o.deps:
                a = ops[d]
                if a.is_dma:
                    continue
                if a.eng == o.eng and not o.is_dma:
                    if a.eng == "pe" or not self.same_engine_sync:
                        continue
                a.sig = True
        cnt = {e: 0 for e in self.ENGS}
        for o in ops:
            if o.sig and not o.is_dma:
                cnt[o.eng] += 1
                o.sigidx = cnt[o.eng]
        with contextlib.ExitStack() as st:
            esem = {e: st.enter_context(nc.semaphore("s_" + e)) for e in self.ENGS}
            dsem = {k: st.enter_context(nc.semaphore("d_%d" % i)) for i, k in enumerate(self.dma_counts)}
            block = st.enter_context(nc.Block())
            per_eng = {e: [o for o in ops if o.eng == e] for e in self.ENGS}

            def run(e, engine):
                known = {}
                for o in per_eng[e]:
                    waits = {}
                    for d in o.deps:
                        a = ops[d]
                        if a.is_dma:
                            s, v = dsem[a.dkey], 16 * a.dcount
                        else:
                            if not a.sig:
                                continue
                            if a.eng == o.eng and not o.is_dma and (a.eng == "pe" or not self.same_engine_sync):
                                continue
                            s, v = esem[a.eng], a.sigidx
                        if waits.get(s, 0) < v:
                            waits[s] = v
                    for s, v in waits.items():
                        if known.get(s, 0) >= v:
                            continue
                        engine.wait_ge(s, v)
                        known[s] = v
                    ins = o.fn(engine)
                    if o.is_dma:
                        ins.then_inc(dsem[o.dkey], 16)
                    elif o.sig:
                        ins.then_inc(esem[e], 1)
                if e == final_wait_engine:
                    for k, c in self.dma_counts.items():
                        engine.wait_ge(dsem[k], 16 * c)

            @block.tensor
            def _(eng):
                run("pe", eng)

            @block.scalar
            def _(eng):
                run("act", eng)

            @block.vector
            def _(eng):
                run("dve", eng)

            @block.gpsimd
            def _(eng):
                run("pool", eng)

            @block.sync
            def _(eng):
                run("sp", eng)


S, D, NT = 2048, 1024, 16
DEPTH = 2
ALPHA = float((2 * DEPTH) ** 0.25)
EPS = 1e-5
D_IN = 2472
D_FF, D_FFE, NE = 2816, 3584, 8
LN8 = float(np.log(0.125))
SCALE_MLA = float(96 ** -0.5)

W_NAMES = ["w_in", "ret_gn_w", "mla_q_norm_w", "mla_w_uq", "mla_kv_norm_w", "mla_w_ukv", "mlstm_conv_w",
           "mlstm_conv_b", "mlstm_b_i", "mlstm_b_f", "mlstm_gn_w", "w_out", "ln1_w", "ln1_b", "ffn_w_gate",
           "ffn_w_up", "ffn_w_down", "moe_router", "moe_w_gate", "moe_w_up", "moe_w_down", "ln2_w", "ln2_b"]
W_SHAPES = {
    "w_in": [2, 1024, 2472], "ret_gn_w": [2, 256], "mla_q_norm_w": [2, 256], "mla_w_uq": [2, 256, 768],
    "mla_kv_norm_w": [2, 128], "mla_w_ukv": [2, 128, 1024], "mlstm_conv_w": [2, 4, 512], "mlstm_conv_b": [2, 512],
    "mlstm_b_i": [2, 4], "mlstm_b_f": [2, 4], "mlstm_gn_w": [2, 256], "w_out": [2, 1024, 1024], "ln1_w": [2, 1024],
    "ln1_b": [2, 1024], "ffn_w_gate": [1, 1024, 2816], "ffn_w_up": [1, 1024, 2816], "ffn_w_down": [1, 2816, 1024],
    "moe_router": [1, 1024, 8], "moe_w_gate": [1, 8, 1024, 3584], "moe_w_up": [1, 8, 1024, 3584],
    "moe_w_down": [1, 8, 3584, 1024], "ln2_w": [2, 1024], "ln2_b": [2, 1024]}


def host_consts():
    c = {}
    c["c_ident"] = np.eye(128, dtype=np.float32)
    j = np.arange(128)
    c["c_tri"] = (j[:, None] <= j[None, :]).astype(np.float32)
    m = np.ones((128, 128), np.float32)
    m[64:, :64] = 0.0
    c["c_mlamask"] = m
    t = np.arange(S, dtype=np.float64)
    inv = 10000.0 ** (-np.arange(32, dtype=np.float64) / 32)
    ang = t[None, :] * inv[:, None]
    cosr = np.concatenate([np.cos(ang), np.cos(ang)], 0)
    sinr = np.concatenate([-np.sin(ang), np.sin(ang)], 0)
    c["c_rc64"] = np.concatenate([cosr, cosr], 0).astype(np.float32)
    c["c_rs64"] = np.concatenate([sinr, sinr], 0).astype(np.float32)
    inv = 10000.0 ** (-np.arange(16, dtype=np.float64) / 16)
    ang = t[None, :] * inv[:, None]
    c["c_rc32"] = np.concatenate([np.cos(ang), np.cos(ang)], 0).astype(np.float32)
    c["c_rs32"] = np.concatenate([-np.sin(ang), np.sin(ang)], 0).astype(np.float32)
    lg = -np.log(1.0 - 2.0 ** (-5.0 - np.arange(4, dtype=np.float64)))
    c["c_retsp"] = np.tile(lg[None, :], (128, 1)).astype(np.float32)
    return c


class _Stop(Exception):
    pass


class MK:
    def __init__(self, phases=("mixer", "ffn"), layers=(0, 1), dbg=(), mix_parts=("ret", "mlstm", "mla")):
        self.phases, self.layers, self.dbg, self.mix_parts = phases, layers, dbg, mix_parts
        nc = self.nc = bass.Bass("TRN2", target_bir_lowering=False)
        self.P = Prog(nc)
        self.x_d = nc.dram_tensor("x", [S, D], F32, kind="ExternalInput").ap()
        self.out_d = nc.dram_tensor("out", [S, D], F32, kind="ExternalOutput").ap()
        self.w = {n: nc.dram_tensor(n, W_SHAPES[n], F32, kind="ExternalInput").ap() for n in W_NAMES}
        self.c = {n: nc.dram_tensor(n, list(v.shape), F32, kind="ExternalInput").ap() for n, v in host_consts().items()}
        self.dbg_d = {}
        self.uid = 0

    def sb(self, st, name, shape, dt):
        self.uid += 1
        return st.enter_context(self.nc.sbuf_tensor("%s_%d" % (name, self.uid), shape, dt))

    def ps(self, st, name, shape, dt):
        self.uid += 1
        return st.enter_context(self.nc.psum_tensor("%s_%d" % (name, self.uid), shape, dt))

    def dbg_out(self, name, shape):
        d = self.nc.dram_tensor("dbg_" + name, shape, F32, kind="ExternalOutput").ap()
        self.dbg_d[name] = d
        return d

    def build(self):
        nc, P = self.nc, self.P
        with contextlib.ExitStack() as st:
            self.xres = self.sb(st, "xres", [128, NT, D], F32)
            self.xT = self.sb(st, "xT", [128, 8, S], BF16)
            self.ident32 = self.sb(st, "ident32", [128, 128], F32)
            self.identb = self.sb(st, "identb", [128, 128], BF16)
            self.tri32 = self.sb(st, "tri32", [128, 128], F32)
            self.trib = self.sb(st, "trib", [128, 128], BF16)
            self.ones32 = self.sb(st, "ones32", [128, 128], F32)
            self.gate = self.sb(st, "gate", [128, NT, NE], F32)
            P.dma("sp", "c0", lambda e: e.dma_start(out=self.ident32[:], in_=self.c["c_ident"]), writes=["ident32"])
            P.dma("sp", "c1", lambda e: e.dma_start(out=self.tri32[:], in_=self.c["c_tri"]), writes=["tri32"])
            P.dma("pool", "c2", lambda e: e.dma_start(out=self.identb[:], in_=self.c["c_ident"]), writes=["identb"])
            P.dma("pool", "c3", lambda e: e.dma_start(out=self.trib[:], in_=self.c["c_tri"]), writes=["trib"])
            P.pool(lambda e: e.memset(self.ones32[:], 1.0), writes=["ones32"])
            self.epsc = self.sb(st, "epsc", [128, 1], F32)
            P.pool(lambda e: e.memset(self.epsc[:], EPS), writes=["epsc"])
            xv = self.x_d.rearrange("(t p) d -> p t d", p=128)
            for g in range(4):
                P.dma("sp", ("xl", g), lambda e, g=g: e.dma_start(out=self.xres[:, 4 * g:4 * g + 4, :], in_=xv[:, 4 * g:4 * g + 4, :]),
                      writes=[("xres", t) for t in range(4 * g, 4 * g + 4)])
            with contextlib.ExitStack() as s2:
                self.finish_setup(s2)
                self.skew([lambda t: self.finish_a(t, self.xres[:, t, :], [("xres", t)]), self.finish_b], NT)
            P.barrier()
            self.stopped = False
            for l in self.layers:
                if "mixer" in self.phases:
                    self.mixer(l)
                    if self.stopped:
                        break
                self.ln_phase(l, 1, router=(l % 2 == 1), last=False)
                if "ffn" in self.phases:
                    self.ffn(l)
                self.ln_phase(l, 2, router=False, last=(l == self.layers[-1]))
            P.emit()
        return nc

    def finish_setup(self, st, nxb=3):
        self.f_xb = [self.sb(st, "f_xb", [128, D], BF16) for _ in range(nxb)]
        self.f_pt = [self.ps(st, "f_pt", [128, 8, 128], BF16) for _ in range(2)]

    def finish_a(self, t, src, src_keys):
        P = self.P
        b = t % len(self.f_xb)
        xb = self.f_xb[b]
        P.act(lambda e: e.copy(out=xb[:], in_=src), reads=src_keys, writes=[("f_xb", b)])
        P.act(lambda e: e.activation(out=self.xres[:, t, :], in_=src, func=AF.Copy, scale=ALPHA),
              reads=src_keys, writes=[("xres", t)])

    def finish_b(self, t):
        P = self.P
        b = t % len(self.f_xb)
        pb = t % 2
        xb, pt = self.f_xb[b], self.f_pt[pb]
        for k in range(8):
            P.pe(lambda e, k=k: e.transpose(out=pt[:, k, :], in_=xb[:, k * 128:(k + 1) * 128], identity=self.identb[:]),
                 reads=[("f_xb", b), "identb"], writes=[("f_pt", pb)])
        P.dve(lambda e: e.tensor_copy(out=self.xT[:, :, t * 128:(t + 1) * 128], in_=pt[:]),
              reads=[("f_pt", pb)], writes=[("xT", t)])

    def finish_tile(self, t, src, src_keys):
        self.finish_a(t, src, src_keys)
        self.finish_b(t)

    @staticmethod
    def skew(stages, n):
        for step in range(n + len(stages) - 1):
            for sidx in range(len(stages) - 1, -1, -1):
                i = step - sidx
                if 0 <= i < n:
                    stages[sidx](i)

    def ln_phase(self, l, which, router, last):
        P, nc = self.P, self.nc
        with contextlib.ExitStack() as st:
            wt = self.sb(st, "ln_w", [128, D], F32)
            bt = self.sb(st, "ln_b", [128, D], F32)
            wn = "ln%d_w" % which
            bn = "ln%d_b" % which
            P.dma("sp", ("lnw", l, which), lambda e: e.dma_start(out=wt[:], in_=self.w[wn][l].partition_broadcast(128)), writes=["ln_w"])
            P.dma("sp", ("lnb", l, which), lambda e: e.dma_start(out=bt[:], in_=self.w[bn][l].partition_broadcast(128)), writes=["ln_b"])
            self.finish_setup(st)
            NS, NM, NX = 3, 4, 4
            stats = [self.sb(st, "ln_st", [128, 2, 6], F32) for _ in range(NS)]
            mv = [self.sb(st, "ln_mv", [128, 4], F32) for _ in range(NM)]
            xn = [self.sb(st, "ln_xn", [128, D], F32) for _ in range(NX)]
            if router:
                rw = self.sb(st, "rw", [128, 8, NE], F32)
                P.dma("sp", ("rw", l), lambda e: e.dma_start(out=rw[:], in_=self.w["moe_router"][l // 2].rearrange("(k p) n -> p k n", p=128)), writes=["rw"])
                xt32 = [self.sb(st, "xt32", [128, 8, 128], F32) for _ in range(2)]
                pt32 = [self.ps(st, "pt32", [128, 4, 128], F32) for _ in range(2)]
                plog = [self.ps(st, "plog", [128, NE], F32) for _ in range(2)]
                rs = [self.sb(st, "rs", [128, 40], F32) for _ in range(2)]

            def a0(t):
                sta, m = stats[t % NS], mv[t % NM]
                y = self.xres[:, t, :]
                for h in range(2):
                    P.dve(lambda e, h=h: e.bn_stats(out=sta[:, h, :], in_=y[:, h * 512:(h + 1) * 512]),
                          reads=[("xres", t)], writes=[("ln_st", t % NS)])
                P.dve(lambda e: e.bn_aggr(out=m[:, 0:2], in_=sta[:].rearrange("p a b -> p (a b)")), reads=[("ln_st", t % NS)], writes=[("ln_mv", t % NM)])

            def a1(t):
                m = mv[t % NM]
                km = [("ln_mv", t % NM)]
                P.act(lambda e: e.activation(out=m[:, 2:3], in_=m[:, 1:2], func=AF.Ln, bias=self.epsc[:], scale=1.0), reads=km + ["epsc"], writes=km)
                P.act(lambda e: e.activation(out=m[:, 2:3], in_=m[:, 2:3], func=AF.Exp, scale=-0.5), reads=km, writes=km)
                P.dve(lambda e: e.scalar_tensor_tensor(out=m[:, 3:4], in0=m[:, 0:1], scalar=-1.0, in1=m[:, 2:3], op0=ALU.mult, op1=ALU.mult), reads=km, writes=km)

            def a2(t):
                m, x_n = mv[t % NM], xn[t % NX]
                kx = [("ln_xn", t % NX)]
                P.act(lambda e: e.activation(out=x_n[:], in_=self.xres[:, t, :], func=AF.Identity, bias=m[:, 3:4], scale=m[:, 2:3]),
                      reads=[("xres", t), ("ln_mv", t % NM)], writes=kx)
                P.dve(lambda e: e.tensor_tensor(out=x_n[:], in0=x_n[:], in1=wt[:], op=ALU.mult), reads=kx + ["ln_w"], writes=kx)
                P.dve(lambda e: e.tensor_tensor(out=x_n[:], in0=x_n[:], in1=bt[:], op=ALU.add), reads=kx + ["ln_b"], writes=kx)

            def a3(t):
                x_n = xn[t % NX]
                kx = [("ln_xn", t % NX)]
                if last:
                    P.dma("sp", ("out", t), lambda e: e.dma_start(out=self.out_d[t * 128:(t + 1) * 128, :], in_=x_n[:]), reads=kx)
                    return
                self.finish_a(t, x_n[:], kx)
                if router:
                    b = t % 2
                    self.router_tile(t, x_n, rw, xt32[b], pt32, plog[b], rs[b], b, xkey=kx[0])

            def a4(t):
                if not last:
                    self.finish_b(t)

            self.skew([a0, a1, a2, a3, a4], NT)
        P.barrier()

    def router_tile(self, t, x_n, rw, xt32, pt32, plog, rs, b, xkey=None):
        P = self.P
        xkey = xkey or ("ln_xn", b)
        for hh in range(2):
            for k in range(4):
                kk = hh * 4 + k
                P.pe(lambda e, k=k, kk=kk, hh=hh: e.transpose(out=pt32[hh][:, k, :], in_=x_n[:, kk * 128:(kk + 1) * 128], identity=self.ident32[:]),
                     reads=[xkey, "ident32"], writes=[("pt32", hh)])
            P.act(lambda e, hh=hh: e.copy(out=xt32[:, hh * 4:hh * 4 + 4, :], in_=pt32[hh][:]), reads=[("pt32", hh)], writes=[("xt32", b)])
        for k in range(8):
            P.pe(lambda e, k=k: e.matmul(plog[:], lhsT=xt32[:, k, :], rhs=rw[:, k, :], start=(k == 0), stop=(k == 7)),
                 reads=[("xt32", b), "rw"], writes=[("plog", b)])
        K = [("rs", b)]
        lg = rs[:, 32:40]
        P.act(lambda e: e.copy(out=lg, in_=plog[:]), reads=[("plog", b)], writes=K)
        P.dve(lambda e: e.reduce_max(out=rs[:, 0:1], in_=lg, axis=AX.X), reads=K, writes=K)
        P.dve(lambda e: e.tensor_scalar(out=rs[:, 8:16], in0=lg, scalar1=rs[:, 0:1], scalar2=None, op0=ALU.is_equal), reads=K, writes=K)
        P.dve(lambda e: e.scalar_tensor_tensor(out=rs[:, 16:24], in0=rs[:, 8:16], scalar=-1e30, in1=lg, op0=ALU.mult, op1=ALU.add), reads=K, writes=K)
        P.dve(lambda e: e.reduce_max(out=rs[:, 1:2], in_=rs[:, 16:24], axis=AX.X), reads=K, writes=K)
        P.dve(lambda e: e.tensor_scalar(out=rs[:, 24:32], in0=rs[:, 16:24], scalar1=rs[:, 1:2], scalar2=None, op0=ALU.is_equal), reads=K, writes=K)
        P.dve(lambda e: e.tensor_tensor(out=rs[:, 2:3], in0=rs[:, 1:2], in1=rs[:, 0:1], op=ALU.subtract), reads=K, writes=K)
        P.act(lambda e: e.activation(out=rs[:, 3:4], in_=rs[:, 2:3], func=AF.Exp), reads=K, writes=K)
        P.dve(lambda e: e.tensor_scalar(out=rs[:, 4:5], in0=rs[:, 3:4], scalar1=1.0, scalar2=None, op0=ALU.add), reads=K, writes=K)
        P.dve(lambda e: e.reciprocal(out=rs[:, 4:5], in_=rs[:, 4:5]), reads=K, writes=K)
        P.dve(lambda e: e.tensor_tensor(out=rs[:, 5:6], in0=rs[:, 3:4], in1=rs[:, 4:5], op=ALU.mult), reads=K, writes=K)
        P.dve(lambda e: e.tensor_scalar(out=self.gate[:, t, :], in0=rs[:, 8:16], scalar1=rs[:, 4:5], scalar2=None, op0=ALU.mult),
              reads=K, writes=[("gate", t)])
        P.dve(lambda e: e.scalar_tensor_tensor(out=self.gate[:, t, :], in0=rs[:, 24:32], scalar=rs[:, 5:6], in1=self.gate[:, t, :], op0=ALU.mult, op1=ALU.add),
              reads=K + [("gate", t)], writes=[("gate", t)])

    def ffn(self, l):
        P = self.P
        if l % 2 == 0:
            F = D_FF
            experts = [(self.w["ffn_w_gate"][l // 2], self.w["ffn_w_up"][l // 2], self.w["ffn_w_down"][l // 2], None)]
        else:
            F = D_FFE
            experts = [(self.w["moe_w_gate"][l // 2][e], self.w["moe_w_up"][l // 2][e], self.w["moe_w_down"][l // 2][e], e) for e in range(NE)]
        nchunks = F // 128
        with contextlib.ExitStack() as st:
            NB = 2
            wgb = [self.sb(st, "wgb", [128, 8, 512], BF16) for _ in range(NB)]
            wub = [self.sb(st, "wub", [128, 8, 512], BF16) for _ in range(NB)]
            wdb = [self.sb(st, "wdb", [128, 4, D], BF16) for _ in range(NB)]
            hT = [self.sb(st, "hT", [128, 4, S], BF16) for _ in range(2)]
            sg = [self.sb(st, "sg", [128, 512], F32) for _ in range(2)]
            pg = [self.ps(st, "pg", [128, 512], F32) for _ in range(2)]
            pu = [self.ps(st, "pu", [128, 512], F32) for _ in range(2)]
            pd = [self.ps(st, "pd", [128, 512], F32) for _ in range(4)]
            gi = 0
            xi = 0
            yi = 0
            for (wg, wu, wd, ei) in experts:
                wgv = wg.rearrange("(k p) n -> p k n", p=128)
                wuv = wu.rearrange("(k p) n -> p k n", p=128)
                wdv = wd.rearrange("(c p) n -> p c n", p=128)
                c0 = 0
                while c0 < nchunks:
                    nch = min(4, nchunks - c0)
                    b = gi % NB
                    hb = gi % 2
                    P.dma("pool", ("wg", b), lambda e, b=b, c0=c0, nch=nch, wgv=wgv: e.dma_start(out=wgb[b][:, :, 0:nch * 128], in_=wgv[:, :, c0 * 128:(c0 + nch) * 128]),
                          writes=[("wgb", b)])
                    P.dma("pool", ("wu", b), lambda e, b=b, c0=c0, nch=nch, wuv=wuv: e.dma_start(out=wub[b][:, :, 0:nch * 128], in_=wuv[:, :, c0 * 128:(c0 + nch) * 128]),
                          writes=[("wub", b)])
                    P.dma("pool", ("wd", b), lambda e, b=b, c0=c0, nch=nch, wdv=wdv: e.dma_start(out=wdb[b][:, 0:nch, :], in_=wdv[:, c0:c0 + nch, :]),
                          writes=[("wdb", b)])
                    for j in range(nch):
                        for t in range(4):
                            x = xi % 2
                            xi += 1
                            xk = [("xT", tt) for tt in range(4 * t, 4 * t + 4)]
                            for k in range(8):
                                P.pe(lambda e, k=k, j=j, t=t, x=x, b=b: e.matmul(pg[x][:], lhsT=wgb[b][:, k, j * 128:(j + 1) * 128], rhs=self.xT[:, k, t * 512:(t + 1) * 512], start=(k == 0), stop=(k == 7)),
                                     reads=[("wgb", b)] + xk, writes=[("pg", x)])
                            for k in range(8):
                                P.pe(lambda e, k=k, j=j, t=t, x=x, b=b: e.matmul(pu[x][:], lhsT=wub[b][:, k, j * 128:(j + 1) * 128], rhs=self.xT[:, k, t * 512:(t + 1) * 512], start=(k == 0), stop=(k == 7)),
                                     reads=[("wub", b)] + xk, writes=[("pu", x)])
                            P.act(lambda e, x=x: e.activation(out=sg[x][:], in_=pg[x][:], func=AF.Silu), reads=[("pg", x)], writes=[("sg", x)])
                            P.dve(lambda e, x=x, j=j, t=t, hb=hb: e.tensor_tensor(out=hT[hb][:, j, t * 512:(t + 1) * 512], in0=sg[x][:], in1=pu[x][:], op=ALU.mult),
                                  reads=[("sg", x), ("pu", x)], writes=[("hT", hb, j, t)])
                    for tile in range(NT):
                        for half in range(2):
                            y = yi % 4
                            yi += 1
                            for j in range(nch):
                                P.pe(lambda e, j=j, tile=tile, half=half, y=y, b=b, hb=hb, nch=nch: e.matmul(pd[y][:], lhsT=hT[hb][:, j, tile * 128:(tile + 1) * 128], rhs=wdb[b][:, j, half * 512:(half + 1) * 512], start=(j == 0), stop=(j == nch - 1)),
                                     reads=[("hT", hb, j, tile // 4), ("wdb", b)], writes=[("pd", y)])
                            xr = self.xres[:, tile, half * 512:(half + 1) * 512]
                            if ei is None:
                                P.dve(lambda e, y=y, xr=xr: e.tensor_tensor(out=xr, in0=xr, in1=pd[y][:], op=ALU.add),
                                      reads=[("pd", y), ("xres", tile)], writes=[("xres", tile)])
                            else:
                                P.dve(lambda e, y=y, xr=xr, tile=tile, ei=ei: e.scalar_tensor_tensor(out=xr, in0=pd[y][:], scalar=self.gate[:, tile, ei:ei + 1], in1=xr, op0=ALU.mult, op1=ALU.add),
                                      reads=[("pd", y), ("xres", tile), ("gate", tile)], writes=[("xres", tile)])
                    c0 += nch
                    gi += 1
        P.barrier()


def make_in_maps(inputs, cores):
    consts = host_consts()
    shared = {n: np.ascontiguousarray(np.asarray(inputs[n], dtype=np.float32)) for n in W_NAMES}
    maps = []
    for c in cores:
        m = {"x": np.ascontiguousarray(np.asarray(inputs["x"][c], dtype=np.float32))}
        m.update(shared)
        m.update(consts)
        maps.append(m)
    return maps


def kernel(**inputs):
    mk = MK()
    nc = mk.build()
    res = run_bass_kernel_spmd(nc, make_in_maps(inputs, list(range(8))), core_ids=list(range(8)))
    return np.stack([np.asarray(r["out"], dtype=np.float32) for r in res.results], 0)


def _mixer(self, l):
    with contextlib.ExitStack() as st:
        self.mergedT = self.sb(st, "mergedT", [128, 8, S], BF16)
        if "dbg_merged" in self.dbg:
            self.P.pool(lambda e: e.memset(self.mergedT[:], 0.0), writes=[("mT", c, t) for c in range(8) for t in range(NT)])
        if "ret" in self.mix_parts:
            self.phase_ret(l)
        if "mlstm" in self.mix_parts:
            self.phase_mlstm(l)
        if "mla" in self.mix_parts:
            self.phase_mla(l)
        if "dbg_merged" in self.dbg:
            d = self.dbg_out("merged%d" % l, [128, 8, S])
            with contextlib.ExitStack() as s2:
                t32 = self.sb(s2, "dbgm", [128, 8, S // 4], F32)
                for q in range(4):
                    self.P.act(lambda e, q=q: e.copy(out=t32[:], in_=self.mergedT[:, :, q * 512:(q + 1) * 512]),
                               reads=[("mT", c, t) for c in range(8) for t in range(NT)], writes=["dbgm"])
                    self.P.dma("sp", ("dbgm", q), lambda e, q=q: e.dma_start(out=d[:, :, q * 512:(q + 1) * 512], in_=t32[:]), reads=["dbgm"])
            self.P.barrier()
            if "stop" in self.dbg:
                self.stopped = True
                return
        self.phase_wout(l)


def _phase_wout(self, l):
    P = self.P
    with contextlib.ExitStack() as st:
        wo = self.sb(st, "wo", [128, 8, D], BF16)
        P.dma("pool", ("wo", l), lambda e: e.dma_start(out=wo[:], in_=self.w["w_out"][l].rearrange("(k p) n -> p k n", p=128)), writes=["wo"])
        pw = [self.ps(st, "pw", [128, 512], F32) for _ in range(4)]
        yi = 0
        for tile in range(NT):
            for half in range(2):
                y = yi % 4
                yi += 1
                for k in range(8):
                    P.pe(lambda e, k=k, tile=tile, half=half, y=y: e.matmul(pw[y][:], lhsT=self.mergedT[:, k, tile * 128:(tile + 1) * 128], rhs=wo[:, k, half * 512:(half + 1) * 512], start=(k == 0), stop=(k == 7)),
                         reads=["wo"] + [("mT", c, tile) for c in range(8)], writes=[("pw", y)])
                xr = self.xres[:, tile, half * 512:(half + 1) * 512]
                P.dve(lambda e, y=y, xr=xr: e.tensor_tensor(out=xr, in0=xr, in1=pw[y][:], op=ALU.add),
                      reads=[("pw", y), ("xres", tile)], writes=[("xres", tile)])
    P.barrier()


def _phase_ret(self, l):
    P = self.P
    with contextlib.ExitStack() as st:
        wR = self.sb(st, "wR", [128, 8, 1024], BF16)
        P.dma("pool", ("wR", l), lambda e: e.dma_start(out=wR[:], in_=self.w["w_in"][l][:, 0:1024].rearrange("(k p) n -> p k n", p=128)), writes=["wR"])
        q32 = self.sb(st, "q32", [128, 2, S], BF16)
        k32 = self.sb(st, "k32", [128, 2, S], BF16)
        with contextlib.ExitStack() as s2:
            wRsw = self.sb(s2, "wRsw", [128, 8, 512], BF16)
            v = wR[:, :, 0:512].rearrange("p k (h t j) -> p k h t j", t=2, j=32)
            vs = wRsw[:].rearrange("p k (h t j) -> p k h t j", t=2, j=32)
            P.act(lambda e: e.copy(out=vs[:, :, :, 0, :], in_=v[:, :, :, 1, :]), reads=["wR"], writes=["wRsw"])
            P.act(lambda e: e.copy(out=vs[:, :, :, 1, :], in_=v[:, :, :, 0, :]), reads=["wR"], writes=["wRsw"])
            rc = [self.sb(s2, "rc", [128, 512], F32) for _ in range(2)]
            rs = [self.sb(s2, "rs", [128, 512], F32) for _ in range(2)]
            tmp = [self.sb(s2, "rtmp", [128, 512], F32) for _ in range(2)]
            tmp2 = [self.sb(s2, "rtmp2", [128, 512], F32) for _ in range(2)]
            pa = [self.ps(s2, "pa", [128, 512], F32) for _ in range(2)]
            pb = [self.ps(s2, "pb", [128, 512], F32) for _ in range(2)]
            xi = 0
            for t in range(4):
                tb = t % 2
                P.dma("sp", ("rc", tb), lambda e, t=t, tb=tb: e.dma_start(out=rc[tb][:], in_=self.c["c_rc64"][:, t * 512:(t + 1) * 512]), writes=[("rc", tb)])
                P.dma("sp", ("rs", tb), lambda e, t=t, tb=tb: e.dma_start(out=rs[tb][:], in_=self.c["c_rs64"][:, t * 512:(t + 1) * 512]), writes=[("rs", tb)])
                xk = [("xT", tt) for tt in range(4 * t, 4 * t + 4)]
                for c in range(4):
                    x = xi % 2
                    xi += 1
                    for k in range(8):
                        P.pe(lambda e, k=k, c=c, t=t, x=x: e.matmul(pa[x][:], lhsT=wR[:, k, c * 128:(c + 1) * 128], rhs=self.xT[:, k, t * 512:(t + 1) * 512], start=(k == 0), stop=(k == 7)),
                             reads=["wR"] + xk, writes=[("pa", x)])
                    for k in range(8):
                        P.pe(lambda e, k=k, c=c, t=t, x=x: e.matmul(pb[x][:], lhsT=wRsw[:, k, c * 128:(c + 1) * 128], rhs=self.xT[:, k, t * 512:(t + 1) * 512], start=(k == 0), stop=(k == 7)),
                             reads=["wRsw"] + xk, writes=[("pb", x)])
                    dst = (q32 if c < 2 else k32)[:, c % 2, t * 512:(t + 1) * 512]
                    dk = ("qk32", c, t)
                    P.dve(lambda e, x=x, tb=tb: e.tensor_tensor(out=tmp[x][:], in0=pa[x][:], in1=rc[tb][:], op=ALU.mult),
                          reads=[("pa", x), ("rc", tb)], writes=[("rtmp", x)])
                    P.dve(lambda e, x=x, tb=tb: e.tensor_tensor(out=tmp2[x][:], in0=pb[x][:], in1=rs[tb][:], op=ALU.mult),
                          reads=[("pb", x), ("rs", tb)], writes=[("rtmp2", x)])
                    P.dve(lambda e, x=x, dst=dst: e.tensor_tensor(out=dst, in0=tmp2[x][:], in1=tmp[x][:], op=ALU.add),
                          reads=[("rtmp2", x), ("rtmp", x)], writes=[dk])
        P.barrier()
        self.linattn(st, l, "ret", q32, k32, lambda k: wR[:, k, 512:1024], None, 0, self.w["ret_gn_w"][l])
    P.barrier()


MK.mixer = _mixer
MK.phase_wout = _phase_wout
MK.phase_ret = _phase_ret


def _linattn(self, st, l, kind, q32, k32, wtok, wgate, chunk0, gnw_ap, bibf=None):
    P = self.P
    ml = kind == "mlstm"

    class Ring:
        def __init__(ring, name, shp, dt, n):
            ring.name, ring.n = name, n
            ring.t = [self.sb(st, kind + name, shp, dt) for _ in range(n)]

        def __call__(ring, i):
            return ring.t[i % ring.n]

        def k(ring, i):
            return (kind, ring.name, i % ring.n)

    vaug = Ring("vaug", [128, 4, 65], BF16, 6)
    sgt = Ring("sgt", [128, 256], F32, 6)
    gcp = Ring("gcp", [128, 256], F32, 1 if ml else 6)
    gt = Ring("gt", [128, 8], F32, 3)
    spt = Ring("spt", [128, 4], F32, 3)
    ut = Ring("ut", [128, 4], F32, 2)
    sprep = Ring("sprep", [128, 4, 64], F32, 2)
    urep = Ring("urep", [128, 4, 64], F32, 2)
    eBU = Ring("eBU", [128, 4, 128], F32, 2)
    eg = Ring("eg", [128, 4], F32, 5)
    qs = Ring("qs", [128, 2, 2, 128], BF16, 3)
    ks = Ring("ks", [128, 2, 128], BF16, 2)
    ktok = Ring("ktok", [128, 256], BF16, 2)
    cm = Ring("cm", [128, 4, 128], BF16, 2)
    yb = Ring("y", [128, 4, 64], F32, 2)
    ysqr = Ring("ysq", [128, 4, 64], F32, 2)
    s8 = Ring("s8", [128, 20], F32, 2)
    zb = Ring("zb", [128, 256], BF16, 2)
    sbf = lambda n, shp, dt: self.sb(st, kind + n, shp, dt)
    S32 = sbf("S32", [128, 2, 65], F32)
    Sb = sbf("Sb", [128, 2, 65], BF16)
    stmp = sbf("stmp", [128, 2, 65], F32)
    gnw = sbf("gnw", [128, 256], F32)
    cst = sbf("cst", [128, 2], F32)
    pv = self.ps(st, "pv", [128, 512], F32)
    pBU = self.ps(st, "pBU", [128, 4, 128], F32)
    psc = self.ps(st, "psc", [128, 4, 128], F32)
    psm = self.ps(st, "psm", [128, 16], F32)
    pob = self.ps(st, "pob", [128, 260], F32)
    pub = self.ps(st, "pub", [128, 260], F32)
    pbk = self.ps(st, "pbk", [128, 256], BF16)
    pbz = self.ps(st, "pbz", [128, 256], BF16)
    pB, pG, pgt = psm[:, 0:4], psm[:, 4:8], psm[:, 8:16]
    po = pob[:].rearrange("p (h e) -> p h e", e=65)
    pu = pub[:].rearrange("p (c r e) -> p c r e", r=2, e=65)
    ptk = pbk[:].rearrange("p (c m) -> p c m", m=128)
    ptz = pbz[:].rearrange("p (c m) -> p c m", m=128)
    K = lambda n: (kind, n)

    P.dma("sp", (kind, "gnw", l), lambda e: e.dma_start(out=gnw[:], in_=gnw_ap.partition_broadcast(128)), writes=[K("gnw")])
    P.pool(lambda e: e.memset(cst[:, 0:1], LN8), writes=[K("cst")])
    P.pool(lambda e: e.memset(cst[:, 1:2], 1.0), writes=[K("cst")])
    P.pool(lambda e: e.memset(S32[:], 0.0), writes=[K("S32")])
    P.pool(lambda e: e.memset(Sb[:], 0.0), writes=[K("Sb")])
    for b in range(vaug.n):
        P.pool(lambda e, b=b: e.memset(vaug.t[b][:], 1.0), writes=[vaug.k(b)])
    for b in range(qs.n):
        P.pool(lambda e, b=b: e.memset(qs.t[b][:], 0.0), writes=[qs.k(b)])
    if ml:
        bibf_t = sbf("bibf", [128, 8], F32)
        P.dma("sp", (kind, "bi", l), lambda e: e.dma_start(out=bibf_t[:, 0:4], in_=bibf[0].partition_broadcast(128)), writes=[K("bibf")])
        P.dma("sp", (kind, "bf", l), lambda e: e.dma_start(out=bibf_t[:, 4:8], in_=bibf[1].partition_broadcast(128)), writes=[K("bibf")])
    else:
        for b in range(spt.n):
            P.dma("sp", (kind, "sp", b, l), lambda e, b=b: e.dma_start(out=spt.t[b][:], in_=self.c["c_retsp"]), writes=[spt.k(b)])
            P.pool(lambda e, b=b: e.memset(gt.t[b][:], 0.0), writes=[gt.k(b)])

    def t0(i):
        cols = slice(i * 128, (i + 1) * 128)
        xk = [("xT", i)]
        for k in range(8):
            P.pe(lambda e, k=k: e.matmul(pv[:], lhsT=self.xT[:, k, cols], rhs=wtok(k), start=(k == 0), stop=(k == 7)),
                 reads=xk, writes=[K("pv")])
        if ml:
            for k in range(8):
                P.pe(lambda e, k=k: e.matmul(pgt, lhsT=self.xT[:, k, cols], rhs=wgate(k), start=(k == 0), stop=(k == 7)),
                     reads=xk, writes=[K("psm")])
            P.dve(lambda e: e.tensor_tensor(out=gt(i)[:], in0=pgt, in1=bibf_t[:], op=ALU.add), reads=[K("psm"), K("bibf")], writes=[gt.k(i)])
        P.act(lambda e: e.copy(out=vaug(i)[:, :, 0:64], in_=pv[:, 0:256].rearrange("p (h d) -> p h d", d=64)),
              reads=[K("pv")], writes=[vaug.k(i)])
        P.act(lambda e: e.activation(out=sgt(i)[:], in_=pv[:, 256:512], func=AF.Exp, scale=-1.0), reads=[K("pv")], writes=[sgt.k(i)])
        if not ml:
            P.act(lambda e: e.copy(out=gcp(i)[:], in_=pv[:, 256:512]), reads=[K("pv")], writes=[gcp.k(i)])
        P.act(lambda e: e.activation(out=sgt(i)[:], in_=sgt(i)[:], func=AF.Ln, bias=cst[:, 1:2], scale=1.0), reads=[sgt.k(i), K("cst")], writes=[sgt.k(i)])
        P.act(lambda e: e.activation(out=sgt(i)[:], in_=sgt(i)[:], func=AF.Exp, scale=-1.0), reads=[sgt.k(i)], writes=[sgt.k(i)])
        if ml:
            P.act(lambda e: e.activation(out=spt(i)[:], in_=gt(i)[:, 4:8], func=AF.Exp, scale=-1.0), reads=[gt.k(i)], writes=[spt.k(i)])
            P.act(lambda e: e.activation(out=spt(i)[:], in_=spt(i)[:], func=AF.Ln, bias=cst[:, 1:2], scale=1.0), reads=[spt.k(i), K("cst")], writes=[spt.k(i)])

    def t1(i):
        P.pe(lambda e: e.matmul(pB, lhsT=self.tri32[:], rhs=spt(i)[:], start=True, stop=True), reads=["tri32", spt.k(i)], writes=[K("psm")])
        P.pe(lambda e: e.matmul(pG, lhsT=self.ones32[:], rhs=spt(i)[:], start=True, stop=True), reads=["ones32", spt.k(i)], writes=[K("psm")])
        P.dve(lambda e: e.tensor_tensor(out=ut(i)[:], in0=pB, in1=gt(i)[:, 0:4], op=ALU.add), reads=[K("psm"), gt.k(i)], writes=[ut.k(i)])
        P.act(lambda e: e.activation(out=eg(i)[:], in_=pG, func=AF.Exp, scale=-1.0), reads=[K("psm")], writes=[eg.k(i)])
        P.act(lambda e: e.copy(out=sprep(i)[:], in_=spt(i)[:].unsqueeze(2).broadcast_to([128, 4, 64])), reads=[spt.k(i)], writes=[sprep.k(i)])
        P.act(lambda e: e.copy(out=urep(i)[:], in_=ut(i)[:].unsqueeze(2).broadcast_to([128, 4, 64])), reads=[ut.k(i)], writes=[urep.k(i)])

    def t2(i):
        cols = slice(i * 128, (i + 1) * 128)
        for c in range(2):
            P.pe(lambda e, c=c: e.matmul(pBU[:, c, :], lhsT=sprep(i)[:, 2 * c:2 * c + 2, :].rearrange("p a b -> p (a b)"), rhs=self.tri32[:], start=True, stop=True),
                 reads=[sprep.k(i), "tri32"], writes=[K("pBU")])
        for c in range(2):
            P.pe(lambda e, c=c: e.matmul(pBU[:, 2 + c, :], lhsT=urep(i)[:, 2 * c:2 * c + 2, :].rearrange("p a b -> p (a b)"), rhs=self.ident32[:], start=True, stop=True),
                 reads=[urep.k(i), "ident32"], writes=[K("pBU")])
        P.act(lambda e: e.activation(out=eBU(i)[:, 0:2, :], in_=pBU[:, 0:2, :], func=AF.Exp, scale=-1.0), reads=[K("pBU")], writes=[eBU.k(i)])
        P.act(lambda e: e.activation(out=eBU(i)[:, 2:4, :], in_=pBU[:, 2:4, :], func=AF.Exp, bias=cst[:, 0:1], scale=1.0), reads=[K("pBU"), K("cst")], writes=[eBU.k(i)])
        qk_keys = [("qk32", c, i // 4) for c in range(4)]
        for r in range(2):
            rr = slice(r * 64, (r + 1) * 64)
            P.dve(lambda e, r=r, rr=rr: e.tensor_tensor(out=qs(i)[rr, :, r, :], in0=q32[rr, :, cols], in1=eBU(i)[rr, 0:2, :], op=ALU.mult), reads=qk_keys + [eBU.k(i)], writes=[qs.k(i)])
        P.dve(lambda e: e.tensor_tensor(out=ks(i)[:], in0=k32[:, :, cols], in1=eBU(i)[:, 2:4, :], op=ALU.mult), reads=qk_keys + [eBU.k(i)], writes=[ks.k(i)])

    def t3(i):
        for c in range(2):
            P.pe(lambda e, c=c: e.transpose(out=ptk[:, c, :], in_=ks(i)[:, c, :], identity=self.identb[:]), reads=[ks.k(i), "identb"], writes=[K("pbk")])
        for h in range(4):
            c, r = h // 2, h % 2
            P.pe(lambda e, h=h, c=c, r=r: e.matmul(psc[:, h, :], lhsT=ks(i)[:, c, :], rhs=qs(i)[:, c, r, :], start=True, stop=True),
                 reads=[ks.k(i), qs.k(i)], writes=[K("psc")])
        P.act(lambda e: e.copy(out=ktok(i)[:].rearrange("p (c m) -> p c m", m=128), in_=ptk), reads=[K("pbk")], writes=[ktok.k(i)])
        P.dve(lambda e: e.tensor_tensor(out=cm(i)[:], in0=psc[:], in1=self.tri32[:].unsqueeze(1).broadcast_to([128, 4, 128]), op=ALU.mult),
              reads=[K("psc"), "tri32"], writes=[cm.k(i)])

    def t4(i):
        for h in range(4):
            c, r = h // 2, h % 2
            P.pe(lambda e, h=h: e.matmul(po[:, h, :], lhsT=cm(i)[:, h, :], rhs=vaug(i)[:, h, :], start=True, stop=False),
                 reads=[cm.k(i), vaug.k(i)], writes=[K("po")])
            P.pe(lambda e, h=h, c=c, r=r: e.matmul(po[:, h, :], lhsT=qs(i)[:, c, r, :], rhs=Sb[:, c, :], start=False, stop=True),
                 reads=[qs.k(i), K("Sb")], writes=[K("po")])
        for c in range(2):
            P.pe(lambda e, c=c: e.matmul(pu[:, c, :, :].rearrange("p r e -> p (r e)"), lhsT=ktok(i)[:, c * 128:(c + 1) * 128], rhs=vaug(i)[:, 2 * c:2 * c + 2, :].rearrange("p a e -> p (a e)"), start=True, stop=True),
                 reads=[ktok.k(i), vaug.k(i)], writes=[K("pu")])
        for r in range(2):
            rows = slice(r * 64, (r + 1) * 64)
            egb = eg(i)[rows, :].rearrange("p (c r) -> p c r", r=2)[:, :, r].unsqueeze(2).broadcast_to([64, 2, 65])
            P.dve(lambda e, r=r, rows=rows: e.tensor_tensor(out=stmp[rows], in0=S32[rows], in1=pu[rows, :, r, :], op=ALU.add),
                  reads=[K("S32"), K("pu")], writes=[K("stmp")])
            P.dve(lambda e, rows=rows, egb=egb: e.tensor_tensor(out=S32[rows], in0=stmp[rows], in1=egb, op=ALU.mult),
                  reads=[K("stmp"), eg.k(i)], writes=[K("S32")])
            P.act(lambda e, rows=rows: e.copy(out=Sb[rows], in_=S32[rows]), reads=[K("S32")], writes=[K("Sb")])
        y = yb(i)
        s = s8(i)
        KY, KS = [yb.k(i)], [s8.k(i)]
        if ml:
            P.act(lambda e: e.activation(out=s[:, 16:20], in_=po[:, :, 64], func=AF.Abs), reads=[K("po")], writes=KS)
        P.act(lambda e: e.copy(out=y[:], in_=po[:, :, 0:64]), reads=[K("po")], writes=KY)

    def t5(i):
        y = yb(i)
        s = s8(i)
        ysq = ysqr(i)
        KY, KS = [yb.k(i)], [s8.k(i)]
        if ml:
            P.dve(lambda e: e.tensor_scalar(out=s[:, 16:20], in0=s[:, 16:20], scalar1=1.0, scalar2=None, op0=ALU.max), reads=KS, writes=KS)
            P.dve(lambda e: e.reciprocal(out=s[:, 16:20], in_=s[:, 16:20]), reads=KS, writes=KS)
            P.dve(lambda e: e.tensor_tensor(out=y[:], in0=y[:], in1=s[:, 16:20].unsqueeze(2).broadcast_to([128, 4, 64]), op=ALU.mult), reads=KY + KS, writes=KY)
            P.dve(lambda e: e.tensor_tensor(out=y[:], in0=y[:], in1=sgt(i)[:].rearrange("p (h d) -> p h d", d=64), op=ALU.mult), reads=KY + [sgt.k(i)], writes=KY)
        P.act(lambda e: e.activation(out=ysq[:], in_=y[:], func=AF.Square), reads=KY, writes=[ysqr.k(i)])
        P.dve(lambda e: e.reduce_sum(out=s[:, 0:4], in_=y[:], axis=AX.X), reads=KY, writes=KS)
        P.dve(lambda e: e.reduce_sum(out=s[:, 4:8], in_=ysq[:], axis=AX.X), reads=[ysqr.k(i)], writes=KS)
        P.dve(lambda e: e.tensor_scalar(out=s[:, 0:4], in0=s[:, 0:4], scalar1=1.0 / 64, scalar2=None, op0=ALU.mult), reads=KS, writes=KS)
        P.dve(lambda e: e.tensor_tensor(out=s[:, 8:12], in0=s[:, 0:4], in1=s[:, 0:4], op=ALU.mult), reads=KS, writes=KS)
        P.dve(lambda e: e.scalar_tensor_tensor(out=s[:, 4:8], in0=s[:, 4:8], scalar=1.0 / 64, in1=s[:, 8:12], op0=ALU.mult, op1=ALU.subtract), reads=KS, writes=KS)
        P.act(lambda e: e.activation(out=s[:, 12:16], in_=s[:, 4:8], func=AF.Ln, bias=self.epsc[:], scale=1.0), reads=KS + ["epsc"], writes=KS)
        P.act(lambda e: e.activation(out=s[:, 12:16], in_=s[:, 12:16], func=AF.Exp, scale=-0.5), reads=KS, writes=KS)
        P.dve(lambda e: e.tensor_tensor(out=y[:], in0=y[:], in1=s[:, 0:4].unsqueeze(2).broadcast_to([128, 4, 64]), op=ALU.subtract), reads=KY + KS, writes=KY)
        P.dve(lambda e: e.tensor_tensor(out=y[:], in0=y[:], in1=s[:, 12:16].unsqueeze(2).broadcast_to([128, 4, 64]), op=ALU.mult), reads=KY + KS, writes=KY)
        yf = y[:].rearrange("p h d -> p (h d)")
        if ml:
            P.dve(lambda e: e.tensor_tensor(out=zb(i)[:], in0=yf, in1=gnw[:], op=ALU.mult), reads=KY + [K("gnw")], writes=[zb.k(i)])
        else:
            P.dve(lambda e: e.tensor_tensor(out=yf, in0=yf, in1=gnw[:], op=ALU.mult), reads=KY + [K("gnw")], writes=KY)
            P.dve(lambda e: e.tensor_tensor(out=yf, in0=yf, in1=gcp(i)[:], op=ALU.mult), reads=KY + [gcp.k(i)], writes=KY)
            P.dve(lambda e: e.tensor_tensor(out=zb(i)[:], in0=yf, in1=sgt(i)[:], op=ALU.mult), reads=KY + [sgt.k(i)], writes=[zb.k(i)])

    def t6(i):
        cols = slice(i * 128, (i + 1) * 128)
        for c in range(2):
            P.pe(lambda e, c=c: e.transpose(out=ptz[:, c, :], in_=zb(i)[:, c * 128:(c + 1) * 128], identity=self.identb[:]), reads=[zb.k(i), "identb"], writes=[K("pbz")])
        P.act(lambda e: e.copy(out=self.mergedT[:, chunk0:chunk0 + 2, cols], in_=ptz), reads=[K("pbz")],
              writes=[("mT", chunk0, i), ("mT", chunk0 + 1, i)])

    stages = [t0, t1, t2, t3, t4, t5, t6]
    for step in range(NT + len(stages) - 1):
        for sidx in range(len(stages) - 1, -1, -1):
            i = step - sidx
            if 0 <= i < NT:
                stages[sidx](i)


MK.linattn = _linattn


def _phase_mlstm(self, l):
    P = self.P
    with contextlib.ExitStack() as st:
        wM = self.sb(st, "wM", [128, 8, 1032], BF16)
        P.dma("pool", ("wM", l), lambda e: e.dma_start(out=wM[:], in_=self.w["w_in"][l][:, 1440:2472].rearrange("(k p) n -> p k n", p=128)), writes=["wM"])
        q32 = self.sb(st, "mq32", [128, 2, S], BF16)
        k32 = self.sb(st, "mk32", [128, 2, S], BF16)
        with contextlib.ExitStack() as s2:
            cw = self.sb(s2, "cw", [128, 4, 4], F32)
            cb = self.sb(s2, "cb", [128, 4], F32)
            for j in range(4):
                P.dma("sp", ("cw", j, l), lambda e, j=j: e.dma_start(out=cw[:, :, j], in_=self.w["mlstm_conv_w"][l][j].rearrange("(c p) -> p c", p=128), allow_slow_non_contiguous=True), writes=["cw"])
            P.dma("sp", ("cb", l), lambda e: e.dma_start(out=cb[:], in_=self.w["mlstm_conv_b"][l].rearrange("(c p) -> p c", p=128), allow_slow_non_contiguous=True), writes=["cb"])
            pre = [self.sb(s2, "pre", [128, 3 + S], F32) for _ in range(2)]
            accs = [self.sb(s2, "cacc", [128, S], F32) for _ in range(2)]
            pa = [self.ps(s2, "mpa", [128, 512], F32) for _ in range(2)]
            for b in range(2):
                P.pool(lambda e, b=b: e.memset(pre[b][:, 0:3], 0.0), writes=[("pre", b)])
            xi = 0
            for c in range(4):
                pb = c % 2
                for t in range(4):
                    x = xi % 2
                    xi += 1
                    xk = [("xT", tt) for tt in range(4 * t, 4 * t + 4)]
                    for k in range(8):
                        P.pe(lambda e, k=k, c=c, t=t, x=x: e.matmul(pa[x][:], lhsT=wM[:, k, c * 128:(c + 1) * 128], rhs=self.xT[:, k, t * 512:(t + 1) * 512], start=(k == 0), stop=(k == 7)),
                             reads=["wM"] + xk, writes=[("mpa", x)])
                    P.act(lambda e, x=x, t=t, pb=pb: e.copy(out=pre[pb][:, 3 + t * 512:3 + (t + 1) * 512], in_=pa[x][:]), reads=[("mpa", x)], writes=[("pre", pb)])
                dst = (q32 if c < 2 else k32)[:, c % 2, :]
                dk = [("qk32", c, t) for t in range(4)]
                acc = accs[pb]
                ak = [("cacc", pb)]
                P.dve(lambda e, c=c, pb=pb, acc=acc: e.tensor_scalar(out=acc[:], in0=pre[pb][:, 0:S], scalar1=cw[:, c, 0:1], scalar2=None, op0=ALU.mult),
                      reads=[("pre", pb), "cw"], writes=ak)
                for j in range(1, 4):
                    P.dve(lambda e, c=c, j=j, pb=pb, acc=acc: e.scalar_tensor_tensor(out=acc[:], in0=pre[pb][:, j:j + S], scalar=cw[:, c, j:j + 1], in1=acc[:], op0=ALU.mult, op1=ALU.add),
                          reads=[("pre", pb), "cw"] + ak, writes=ak)
                P.act(lambda e, c=c, dst=dst, acc=acc: e.activation(out=dst, in_=acc[:], func=AF.Silu, bias=cb[:, c:c + 1], scale=1.0), reads=ak + ["cb"], writes=dk)
        P.barrier()
        self.linattn(st, l, "mlstm", q32, k32, lambda k: wM[:, k, 512:1024], lambda k: wM[:, k, 1024:1032], 6,
                     self.w["mlstm_gn_w"][l], (self.w["mlstm_b_i"][l], self.w["mlstm_b_f"][l]))
    P.barrier()


MK.phase_mlstm = _phase_mlstm


def _phase_mla(self, l):
    P = self.P
    with contextlib.ExitStack() as st:
        wq_pad = self.sb(st, "wq_pad", [128, 2, 8, 128], BF16)
        wq_sw = self.sb(st, "wq_sw", [128, 2, 8, 32], BF16)
        wk_pad = self.sb(st, "wk_pad", [128, 8, 128], BF16)
        wv_pad = self.sb(st, "wv_pad", [128, 8, 64], BF16)
        nw = self.sb(st, "nw", [128, 4], F32)
        cqn = self.sb(st, "cqn", [128, 2, S], BF16)
        ckvn = self.sb(st, "ckvn", [128, S], BF16)
        krope = self.sb(st, "krope", [32, S], BF16)
        P.dma("sp", ("nwq", l), lambda e: e.dma_start(out=nw[:, 0:2], in_=self.w["mla_q_norm_w"][l].rearrange("(c p) -> p c", p=128), allow_slow_non_contiguous=True), writes=["nw"])
        P.dma("sp", ("nwk", l), lambda e: e.dma_start(out=nw[:, 2:3], in_=self.w["mla_kv_norm_w"][l].rearrange("(c p) -> p c", p=128), allow_slow_non_contiguous=True), writes=["nw"])
        with contextlib.ExitStack() as s2:
            wuq = self.sb(s2, "wuq", [128, 2, 768], BF16)
            wukv = self.sb(s2, "wukv", [128, 1024], BF16)
            P.dma("pool", ("wuq", l), lambda e: e.dma_start(out=wuq[:], in_=self.w["mla_w_uq"][l].rearrange("(k p) n -> p k n", p=128)), writes=["wuq"])
            P.dma("pool", ("wukv", l), lambda e: e.dma_start(out=wukv[:], in_=self.w["mla_w_ukv"][l]), writes=["wukv"])
            uqv = wuq[:].rearrange("p k (h c) -> p k h c", c=96)
            P.pool(lambda e: e.memset(wq_pad[:], 0.0), writes=["wq_pad"])
            P.pool(lambda e: e.tensor_copy(out=wq_pad[:, :, :, 0:32], in_=uqv[:, :, :, 64:96]), reads=["wuq"], writes=["wq_pad"])
            P.pool(lambda e: e.tensor_copy(out=wq_pad[:, :, :, 64:128], in_=uqv[:, :, :, 0:64]), reads=["wuq"], writes=["wq_pad"])
            P.pool(lambda e: e.tensor_copy(out=wq_sw[:, :, :, 0:16], in_=uqv[:, :, :, 80:96]), reads=["wuq"], writes=["wq_sw"])
            P.pool(lambda e: e.tensor_copy(out=wq_sw[:, :, :, 16:32], in_=uqv[:, :, :, 64:80]), reads=["wuq"], writes=["wq_sw"])
            ukv = wukv[:].rearrange("p (h c) -> p h c", c=128)
            P.pool(lambda e: e.memset(wk_pad[:], 0.0), writes=["wk_pad"])
            P.pool(lambda e: e.tensor_copy(out=wk_pad[:, :, 64:128], in_=ukv[:, :, 0:64]), reads=["wukv"], writes=["wk_pad"])
            P.pool(lambda e: e.tensor_copy(out=wv_pad[:], in_=ukv[:, :, 64:128]), reads=["wukv"], writes=["wv_pad"])


            wA = self.sb(s2, "wA", [128, 8, 416], BF16)
            wAsw = self.sb(s2, "wAsw", [128, 8, 32], BF16)
            P.dma("pool", ("wA", l), lambda e: e.dma_start(out=wA[:], in_=self.w["w_in"][l][:, 1024:1440].rearrange("(k p) n -> p k n", p=128)), writes=["wA"])
            P.pool(lambda e: e.tensor_copy(out=wAsw[:, :, 0:16], in_=wA[:, :, 400:416]), reads=["wA"], writes=["wAsw"])
            P.pool(lambda e: e.tensor_copy(out=wAsw[:, :, 16:32], in_=wA[:, :, 384:400]), reads=["wA"], writes=["wAsw"])
            c32 = self.sb(s2, "c32", [128, 3, 512], F32)
            sq = self.sb(s2, "sq", [128, 3, 512], F32)
            rstd = self.sb(s2, "rstd", [128, 2, 512], F32)
            rc = self.sb(s2, "rc32", [32, 512], F32)
            rs = self.sb(s2, "rs32", [32, 512], F32)
            t1 = self.sb(s2, "kt1", [32, 512], F32)
            t2 = self.sb(s2, "kt2", [32, 512], F32)
            pc = [self.ps(s2, "pc", [128, 512], F32) for _ in range(3)]
            pkr = self.ps(s2, "pkr", [32, 512], F32)
            pks = self.ps(s2, "pks", [32, 512], F32)
            pss = [self.ps(s2, "pss", [128, 512], F32) for _ in range(2)]

            def a1(t):
                tg = slice(t * 512, (t + 1) * 512)
                xk = [("xT", tt) for tt in range(4 * t, 4 * t + 4)]
                P.dma("sp", ("rc32", l), lambda e: e.dma_start(out=rc[:], in_=self.c["c_rc32"][:, tg]), writes=["rc32"])
                P.dma("sp", ("rs32", l), lambda e: e.dma_start(out=rs[:], in_=self.c["c_rs32"][:, tg]), writes=["rs32"])
                for c in range(3):
                    for k in range(8):
                        P.pe(lambda e, c=c, k=k: e.matmul(pc[c][:], lhsT=wA[:, k, c * 128:(c + 1) * 128], rhs=self.xT[:, k, tg], start=(k == 0), stop=(k == 7)),
                             reads=["wA"] + xk, writes=[("pc", c)])
                for k in range(8):
                    P.pe(lambda e, k=k: e.matmul(pkr[:], lhsT=wA[:, k, 384:416], rhs=self.xT[:, k, tg], start=(k == 0), stop=(k == 7)), reads=["wA"] + xk, writes=["pkr"])
                for k in range(8):
                    P.pe(lambda e, k=k: e.matmul(pks[:], lhsT=wAsw[:, k, :], rhs=self.xT[:, k, tg], start=(k == 0), stop=(k == 7)), reads=["wAsw"] + xk, writes=["pks"])
                for c in range(3):
                    P.act(lambda e, c=c: e.copy(out=c32[:, c, :], in_=pc[c][:]), reads=[("pc", c)], writes=[("c32", c)])
                    P.act(lambda e, c=c: e.activation(out=sq[:, c, :], in_=pc[c][:], func=AF.Square), reads=[("pc", c)], writes=[("sq", c)])
                for c in range(2):
                    P.pe(lambda e, c=c: e.matmul(pss[0][:], lhsT=self.ones32[:], rhs=sq[:, c, :], start=(c == 0), stop=(c == 1)), reads=[("sq", c), "ones32"], writes=[("pss", 0)])
                P.pe(lambda e: e.matmul(pss[1][:], lhsT=self.ones32[:], rhs=sq[:, 2, :], start=True, stop=True), reads=[("sq", 2), "ones32"], writes=[("pss", 1)])
                for i, n in ((0, 256.0), (1, 128.0)):
                    P.act(lambda e, i=i, n=n: e.activation(out=rstd[:, i, :], in_=pss[i][:], func=AF.Ln, bias=self.epsc[:], scale=1.0 / n), reads=[("pss", i), "epsc"], writes=[("rstd", i)])
                    P.act(lambda e, i=i: e.activation(out=rstd[:, i, :], in_=rstd[:, i, :], func=AF.Exp, scale=-0.5), reads=[("rstd", i)], writes=[("rstd", i)])
                for c in range(2):
                    P.dve(lambda e, c=c: e.scalar_tensor_tensor(out=cqn[:, c, tg], in0=c32[:, c, :], scalar=nw[:, c:c + 1], in1=rstd[:, 0, :], op0=ALU.mult, op1=ALU.mult),
                          reads=[("c32", c), "nw", ("rstd", 0)], writes=[("cqn", t)])
                P.dve(lambda e: e.scalar_tensor_tensor(out=ckvn[:, tg], in0=c32[:, 2, :], scalar=nw[:, 2:3], in1=rstd[:, 1, :], op0=ALU.mult, op1=ALU.mult),
                      reads=[("c32", 2), "nw", ("rstd", 1)], writes=[("ckvn", t)])
                P.dve(lambda e: e.tensor_tensor(out=t1[:], in0=pkr[:], in1=rc[:], op=ALU.mult), reads=["pkr", "rc32"], writes=["kt1"])
                P.dve(lambda e: e.tensor_tensor(out=t2[:], in0=pks[:], in1=rs[:], op=ALU.mult), reads=["pks", "rs32"], writes=["kt2"])
                P.dve(lambda e: e.tensor_tensor(out=krope[:, tg], in0=t1[:], in1=t2[:], op=ALU.add), reads=["kt1", "kt2"], writes=[("krope", t)])

            for t in range(4):
                a1(t)
        P.barrier()

        KT0 = self.sb(st, "KT", [128, 2, S], BF16)
        QT0 = self.sb(st, "QT", [128, 2, S], BF16)
        Vaug0 = self.sb(st, "Vaug", [128, NT, 2, 128], BF16)
        sets = [dict(KT=KT0[:], QT=QT0[:], V=Vaug0[:]),
                dict(KT=self.xT[:, 0:2, :], QT=self.xT[:, 2:4, :],
                     V=self.xT[:, 4:6, :].rearrange("p a (t r d) -> p (a t) r d", r=2, d=128))]
        PT = [self.sb(st, "PT", [128, 512], BF16) for _ in range(2)]
        sqf = [self.sb(st, "sqf", [128, 512], F32) for _ in range(2)]
        dsh = [self.sb(st, "dsh", [128, 512], F32) for _ in range(2)]
        qt1 = self.sb(st, "qt1", [32, 512], F32)
        qt2 = self.sb(st, "qt2", [32, 512], F32)
        rq = self.sb(st, "rq", [64, 512], F32)
        kmxs = [self.sb(st, "kmx", [128, 8], F32) for _ in range(2)]
        mask = self.sb(st, "mlamask", [128, 128], BF16)
        rcf = [self.sb(st, "rcf", [32, 512], F32) for _ in range(2)]
        rsf = [self.sb(st, "rsf", [32, 512], F32) for _ in range(2)]
        P.dma("pool", ("mask", l), lambda e: e.dma_start(out=mask[:], in_=self.c["c_mlamask"]), writes=["mlamask"])
        for si in range(2):
            P.pool(lambda e, si=si: e.memset(sets[si]["V"], 1.0), writes=[("Vaug", si)])
        pk = [self.ps(st, "pk", [128, 512], F32) for _ in range(2)]
        pbd = [self.ps(st, "pbd", [128, 512], F32) for _ in range(2)]
        pqs = self.ps(st, "pqs", [32, 512], F32)
        pn = self.ps(st, "pn", [128, 512], F32)
        po = [self.ps(st, "pmo", [128, 512], F32) for _ in range(2)]
        cnt = {"sq": 0, "n": 0, "bd": 0, "tb": 0}

        def build(hp):
            si = hp % 2
            KT, QT, Vaug, kmx = sets[si]["KT"], sets[si]["QT"], sets[si]["V"], kmxs[si]
            KK = lambda r, t: ("KT", si, r, t)
            QQ = lambda r, t: ("QT", si, r, t)
            units = [(r, t) for r in range(2) for t in range(4)]

            def slots(stages, n):
                for step in range(n + len(stages) - 1):
                    for sidx in range(len(stages) - 1, -1, -1):
                        u = step - sidx
                        if 0 <= u < n:
                            stages[sidx](u)
                    yield

            kx = [None] * 8
            ksq = [None] * 8

            def k_a(u):
                r, t = units[u]
                h = 2 * hp + r
                tg = slice(t * 512, (t + 1) * 512)
                x = cnt["bd"] % 2
                cnt["bd"] += 1
                kx[u] = x
                P.pe(lambda e: e.matmul(pbd[x][:], lhsT=wk_pad[:, h, :], rhs=ckvn[:, tg], start=True, stop=True), reads=["wk_pad", ("ckvn", t)], writes=[("pbd", x)])

            def k_b(u):
                r, t = units[u]
                tg = slice(t * 512, (t + 1) * 512)
                x = kx[u]
                q = cnt["sq"] % 2
                cnt["sq"] += 1
                ksq[u] = q
                kk = [KK(r, t)]
                P.act(lambda e: e.copy(out=KT[:, r, tg], in_=pbd[x][:]), reads=[("pbd", x)], writes=kk)
                P.act(lambda e: e.copy(out=KT[0:32, r, tg], in_=krope[:, tg]), reads=[("krope", t)] + kk, writes=kk)
                P.pool(lambda e: e.memset(KT[32:33, r, tg], 1.0), reads=kk, writes=kk)
                P.act(lambda e: e.activation(out=sqf[q][:], in_=KT[:, r, tg], func=AF.Square), reads=kk, writes=[("sqf", q)])

            def k_c(u):
                q = ksq[u]
                P.pe(lambda e: e.matmul(pn[:], lhsT=self.ones32[:], rhs=sqf[q][:], start=True, stop=True), reads=[("sqf", q), "ones32"], writes=["pn"])
                r, t = units[u]
                P.dve(lambda e: e.reduce_max(out=kmx[:, r * 4 + t:r * 4 + t + 1], in_=pn[:], axis=AX.X), reads=["pn"], writes=[("kmx", si)])

            yield from slots([k_a, k_b, k_c], 8)
            for r in range(2):
                P.dve(lambda e, r=r: e.reduce_max(out=kmx[:, r * 4:r * 4 + 1], in_=kmx[:, r * 4:r * 4 + 4], axis=AX.X), reads=[("kmx", si)], writes=[("kmx", si)])

            vx = [None] * 4

            def v_a(g):
                x = cnt["bd"] % 2
                cnt["bd"] += 1
                vx[g] = x
                pvv = pbd[x][:].rearrange("p (a d) -> p a d", d=128)
                for ii in range(4):
                    i = 4 * g + ii
                    P.pe(lambda e, i=i, ii=ii: e.matmul(pvv[:, ii, :], lhsT=ckvn[:, i * 128:(i + 1) * 128], rhs=wv_pad[:, 2 * hp:2 * hp + 2, :].rearrange("p a d -> p (a d)"), start=True, stop=True),
                         reads=["wv_pad", ("ckvn", g)], writes=[("pbd", x)])

            def v_b(g):
                x = vx[g]
                pvv = pbd[x][:].rearrange("p (a d) -> p a d", d=128)
                P.act(lambda e: e.copy(out=Vaug[:, 4 * g:4 * g + 4, 0, 0:64], in_=pvv[:, :, 0:64]), reads=[("pbd", x)], writes=[("Vaug", si)])
                P.dve(lambda e: e.tensor_copy(out=Vaug[:, 4 * g:4 * g + 4, 1, 64:128], in_=pvv[:, :, 64:128]), reads=[("pbd", x)], writes=[("Vaug", si)])

            yield from slots([v_a, v_b], 4)

            qx = [None] * 8
            qsq = [None] * 8
            qtb = [None] * 8

            def q_a(u):
                r, t = units[u]
                h = 2 * hp + r
                tg = slice(t * 512, (t + 1) * 512)
                x = cnt["bd"] % 2
                cnt["bd"] += 1
                qx[u] = x
                tb = cnt["tb"] % 2
                cnt["tb"] += 1
                qtb[u] = tb
                P.dma("sp", ("rcf", tb), lambda e: e.dma_start(out=rcf[tb][:], in_=self.c["c_rc32"][:, tg]), writes=[("rcf", tb)])
                P.dma("sp", ("rsf", tb), lambda e: e.dma_start(out=rsf[tb][:], in_=self.c["c_rs32"][:, tg]), writes=[("rsf", tb)])
                for kc in range(2):
                    P.pe(lambda e, kc=kc: e.matmul(pbd[x][:], lhsT=wq_pad[:, kc, h, :], rhs=cqn[:, kc, tg], start=(kc == 0), stop=(kc == 1)), reads=["wq_pad", ("cqn", t)], writes=[("pbd", x)])
                for kc in range(2):
                    P.pe(lambda e, kc=kc: e.matmul(pqs[:], lhsT=wq_sw[:, kc, h, :], rhs=cqn[:, kc, tg], start=(kc == 0), stop=(kc == 1)), reads=["wq_sw", ("cqn", t)], writes=["pqs"])

            def q_b(u):
                r, t = units[u]
                tg = slice(t * 512, (t + 1) * 512)
                x, tb = qx[u], qtb[u]
                QK = [QQ(r, t)]
                P.act(lambda e: e.copy(out=QT[64:128, r, tg], in_=pbd[x][64:128, :]), reads=[("pbd", x)], writes=QK)
                P.act(lambda e: e.copy(out=QT[32:64, r, tg], in_=pbd[x][32:64, :]), reads=[("pbd", x)], writes=QK)
                P.dve(lambda e: e.tensor_tensor(out=qt1[:], in0=pbd[x][0:32, :], in1=rcf[tb][:], op=ALU.mult), reads=[("pbd", x), ("rcf", tb)], writes=["qt1"])
                P.dve(lambda e: e.tensor_tensor(out=qt2[:], in0=pqs[:], in1=rsf[tb][:], op=ALU.mult), reads=["pqs", ("rsf", tb)], writes=["qt2"])
                P.dve(lambda e: e.tensor_tensor(out=QT[0:32, r, tg], in0=qt1[:], in1=qt2[:], op=ALU.add), reads=["qt1", "qt2"], writes=QK)

            def q_c(u):
                r, t = units[u]
                tg = slice(t * 512, (t + 1) * 512)
                q = cnt["sq"] % 2
                cnt["sq"] += 1
                qsq[u] = q
                P.act(lambda e: e.activation(out=sqf[q][:], in_=QT[:, r, tg], func=AF.Square), reads=[QQ(r, t)], writes=[("sqf", q)])

            def q_d(u):
                r, t = units[u]
                q = qsq[u]
                P.pe(lambda e: e.matmul(pn[:], lhsT=self.ones32[:], rhs=sqf[q][:], start=True, stop=True), reads=[("sqf", q), "ones32"], writes=["pn"])
                P.dve(lambda e: e.tensor_scalar(out=rq[32:33, :], in0=pn[32:33, :], scalar1=kmx[32:33, r * 4:r * 4 + 1], scalar2=None, op0=ALU.mult), reads=["pn", ("kmx", si)], writes=["rq"])

            def q_e(u):
                r, t = units[u]
                tg = slice(t * 512, (t + 1) * 512)
                QK = [QQ(r, t)]
                P.act(lambda e: e.activation(out=rq[32:33, :], in_=rq[32:33, :], func=AF.Ln), reads=["rq"], writes=["rq"])
                P.act(lambda e: e.activation(out=rq[32:33, :], in_=rq[32:33, :], func=AF.Exp, scale=0.5), reads=["rq"], writes=["rq"])
                P.dve(lambda e: e.tensor_scalar(out=QT[32:33, r, tg], in0=rq[32:33, :], scalar1=-1.0, scalar2=None, op0=ALU.mult), reads=["rq"] + QK, writes=QK)

            yield from slots([q_a, q_b, q_c, q_d, q_e], 8)

        def attend(hp):
            si = hp % 2
            KT, QT, Vaug = sets[si]["KT"], sets[si]["QT"], sets[si]["V"]
            items = []
            for r in range(2):
                for g in range(4):
                    for j in range(4 * g + 4):
                        items.append((r, g, j))

            def qk(n):
                r, g, j = items[n]
                x = cnt["n"] % 2
                q_lo = max(512 * g, 128 * j)
                N = 512 * (g + 1) - q_lo
                P.pe(lambda e: e.matmul(pk[x][:, 0:N], lhsT=KT[:, r, j * 128:(j + 1) * 128], rhs=QT[:, r, q_lo:q_lo + N], start=True, stop=True),
                     reads=[("KT", si, r, j // 4)] + [("QT", si, r, tt) for tt in range(q_lo // 512, g + 1)], writes=[("pk", x)])
                P.act(lambda e: e.activation(out=PT[x][:, 0:N], in_=pk[x][:, 0:N], func=AF.Exp, scale=SCALE_MLA), reads=[("pk", x)], writes=[("PT", x)])
                if 128 * j >= 512 * g:
                    P.dve(lambda e: e.tensor_tensor(out=PT[x][:, 0:128], in0=PT[x][:, 0:128], in1=mask[:], op=ALU.mult), reads=[("PT", x), "mlamask"], writes=[("PT", x)])
                cnt["n"] += 1
                return x

            def pv(n, x):
                r, g, j = items[n]
                og = (r * 4 + g) % 2
                q_lo = max(512 * g, 128 * j)
                N = 512 * (g + 1) - q_lo
                off = q_lo - 512 * g
                last = (j == 4 * g + 3)
                P.pe(lambda e: e.matmul(po[og][:, off:off + N], lhsT=Vaug[:, j, r, :], rhs=PT[x][:, 0:N], start=(j == 0), stop=last),
                     reads=[("Vaug", si), ("PT", x)], writes=[("pmo", og)])
                if last:
                    nr = slice(r * 64, (r + 1) * 64)
                    dr = slice((1 - r) * 64, (2 - r) * 64)
                    P.act(lambda e: e.activation(out=dsh[og][nr, :], in_=po[og][dr, :], func=AF.Ln), reads=[("pmo", og)], writes=[("dsh", og)])
                    P.act(lambda e: e.activation(out=dsh[og][nr, :], in_=dsh[og][nr, :], func=AF.Exp, scale=-1.0), reads=[("dsh", og)], writes=[("dsh", og)])
                    P.dve(lambda e: e.tensor_tensor(out=self.mergedT[nr, 2 + hp, g * 512:(g + 1) * 512], in0=po[og][nr, :], in1=dsh[og][nr, :], op=ALU.mult),
                          reads=[("pmo", og), ("dsh", og)], writes=[("mT", 2 + hp, tt) for tt in range(4 * g, 4 * g + 4)])

            prev = None
            for n in range(len(items)):
                x = qk(n)
                if prev is not None:
                    pv(*prev)
                prev = (n, x)
                if n % 3 == 2:
                    yield
            pv(*prev)
            yield

        def drain(g):
            for _ in g:
                pass

        drain(build(0))
        for hp in range(4):
            ga = attend(hp)
            gb = build(hp + 1) if hp + 1 < 4 else iter(())
            a_done = b_done = False
            while not (a_done and b_done):
                if not b_done:
                    try:
                        next(gb)
                    except StopIteration:
                        b_done = True
                if not a_done:
                    try:
                        next(ga)
                    except StopIteration:
                        a_done = True
    P.barrier()


MK.phase_mla = _phase_mla
```
